# Optimizing a Trainium2 kernel written in Bass

```python
import math
import jax, jax.numpy as jnp
from jax import lax
import numpy as np

D_MODEL = 1024
BATCH = 16
SEQ = 2048
DEPTH = 2
DEC_BATCH = 128
DEC_SEQ = 8
PAST_LEN = 16384
PAGE_SIZE = 128

N_A_LAYERS = DEPTH // 2
N_B_LAYERS = DEPTH - N_A_LAYERS
N_DENSE_LAYERS = (DEPTH + 1) // 2
N_MOE_LAYERS = DEPTH // 2
D_PLE = 256
D_RNN = 1536
RNN_BLOCK = 128
N_RNN_BLOCKS = D_RNN // RNN_BLOCK
CONV_WIDTH = 4
LRU_C = 8.0
N_HEADS = 16
Q_LORA_RANK = 384
KV_LORA_RANK = 256
QK_NOPE_DIM = 64
QK_ROPE_DIM = 32
V_HEAD_DIM = 64
ROPE_THETA = 10000.0
Q_BLOCK = 128
SOFTMAX_SCALE = (QK_NOPE_DIM + QK_ROPE_DIM) ** -0.5
D_FF = 2816
N_EXPERTS = 8
TOP_K = 2
D_FF_EXPERT = 3584
LN_EPS = 1e-5
RMS_EPS = 1e-6
DEEPNORM_ALPHA = (2.0 * DEPTH) ** 0.25
DEEPNORM_BETA = (8.0 * DEPTH) ** -0.25

kernel_name = 'hybrid_rglru_mla_yoco_step'


def layer_norm(x, g, b):
    xf = x.astype(jnp.float32)
    mu = jnp.mean(xf, -1, keepdims=True)
    var = jnp.mean(jnp.square(xf - mu), -1, keepdims=True)
    return ((xf - mu) * lax.rsqrt(var + LN_EPS) * g + b).astype(x.dtype)


def rms_norm(x, g):
    xf = x.astype(jnp.float32)
    return (xf * lax.rsqrt(jnp.mean(jnp.square(xf), -1, keepdims=True) + RMS_EPS) * g).astype(x.dtype)


def rope_angles(pos):
    inv = ROPE_THETA ** (-jnp.arange(0, QK_ROPE_DIM, 2, dtype=jnp.float32) / QK_ROPE_DIM)
    ang = pos[:, None] * inv[None, :]
    return jnp.cos(ang), jnp.sin(ang)


def apply_rope(x, cos, sin):
    x1, x2 = jnp.split(x.astype(jnp.float32), 2, axis=-1)
    return jnp.concatenate([x1 * cos - x2 * sin, x1 * sin + x2 * cos], axis=-1).astype(x.dtype)


def recurrent_block(x, conv_state, h0, w_gate, w_x, conv_w, conv_b, w_a, b_a, w_i, b_i, lam, w_out):
    B, S, _ = x.shape
    gate = jax.nn.gelu(x @ w_gate)
    u = x @ w_x
    u_ext = jnp.concatenate([conv_state.astype(u.dtype), u], axis=1)
    conv = conv_b
    for k in range(CONV_WIDTH):
        conv = conv + u_ext[:, k:k + S] * conv_w[k]
    new_conv_state = u_ext[:, -(CONV_WIDTH - 1):].astype(conv_state.dtype)
    cb = conv.reshape(B, S, N_RNN_BLOCKS, RNN_BLOCK)
    r = jax.nn.sigmoid((jnp.einsum('bsnd,nde->bsne', cb, w_a).reshape(B, S, D_RNN) + b_a).astype(jnp.float32))
    i = jax.nn.sigmoid((jnp.einsum('bsnd,nde->bsne', cb, w_i).reshape(B, S, D_RNN) + b_i).astype(jnp.float32))
    log_a = -LRU_C * r * jax.nn.softplus(-lam.astype(jnp.float32))
    a = jnp.exp(log_a)
    bterm = jnp.sqrt(1.0 - jnp.exp(2.0 * log_a)) * (i * conv.astype(jnp.float32))
    bterm = bterm.at[:, 0].add(a[:, 0] * h0.astype(jnp.float32))

    def combine(lhs, rhs):
        a1, b1 = lhs
        a2, b2 = rhs
        return a1 * a2, a2 * b1 + b2

    _, h = lax.associative_scan(combine, (a, bterm), axis=1)
    y = (h.astype(x.dtype) * gate) @ w_out
    return y, new_conv_state, h[:, -1].astype(h0.dtype)


def swiglu(x, w_gate, w_up, w_down):
    return (jax.nn.silu(x @ w_gate) * (x @ w_up)) @ w_down


def moe_swiglu(x, w_router, w_gate, w_up, w_down):
    logits = (x @ w_router).astype(jnp.float32)
    top_val, top_idx = lax.top_k(logits, TOP_K)
    top_w = jax.nn.softmax(top_val, axis=-1)
    gates = jnp.sum(jax.nn.one_hot(top_idx, N_EXPERTS, dtype=jnp.float32) * top_w[..., None], axis=-2)
    out = jnp.zeros_like(x)
    for e in range(N_EXPERTS):
        out = out + gates[..., e:e + 1].astype(x.dtype) * swiglu(x, w_gate[e], w_up[e], w_down[e])
    return out


def mla_shared_kv(h, w_kv_a, kv_norm_g, cos, sin):
    kv = h @ w_kv_a
    c_kv = rms_norm(kv[..., :KV_LORA_RANK], kv_norm_g)
    k_pe = apply_rope(kv[..., KV_LORA_RANK:], cos[None], sin[None])
    return c_kv, k_pe


def mla_queries(x, w_q_a, q_norm_g, w_q_b, w_uk, cos, sin):
    B, S, _ = x.shape
    cq = rms_norm(x @ w_q_a, q_norm_g)
    q = (cq @ w_q_b).reshape(B, S, N_HEADS, QK_NOPE_DIM + QK_ROPE_DIM)
    q_nope = q[..., :QK_NOPE_DIM]
    q_pe = apply_rope(q[..., QK_NOPE_DIM:], cos[None, :, None, :], sin[None, :, None, :])
    q_lat = jnp.einsum('bshd,chd->bshc', q_nope, w_uk)
    return q_lat * SOFTMAX_SCALE, q_pe * SOFTMAX_SCALE


def mla_output(o_lat, w_uv, w_o):
    B, S = o_lat.shape[:2]
    v = jnp.einsum('bshc,chv->bshv', o_lat, w_uv)
    return v.reshape(B, S, N_HEADS * V_HEAD_DIM) @ w_o


def latent_attention_prompt(q_lat, q_pe, c_kv, k_pe):
    B, S = q_lat.shape[:2]
    nb = S // Q_BLOCK

    def blockify(t):
        return jnp.moveaxis(t.reshape((B, nb, Q_BLOCK) + t.shape[2:]), 1, 0)

    key_pos = jnp.arange(S)

    def one_block(args):
        ql, qp, start = args
        s = (jnp.einsum('bqhc,bkc->bhqk', ql, c_kv) + jnp.einsum('bqhr,bkr->bhqk', qp, k_pe)).astype(jnp.float32)
        q_pos = start + jnp.arange(Q_BLOCK)
        s = jnp.where(key_pos[None, None, None, :] <= q_pos[None, None, :, None], s, -jnp.inf)
        pr = jax.nn.softmax(s, axis=-1).astype(c_kv.dtype)
        return jnp.einsum('bhqk,bkc->bqhc', pr, c_kv)

    o = lax.map(one_block, (blockify(q_lat), blockify(q_pe), jnp.arange(nb) * Q_BLOCK))
    return jnp.moveaxis(o, 0, 1).reshape(B, S, N_HEADS, KV_LORA_RANK)


def latent_attention_sample(q_lat, q_pe, c_new, kpe_new, cache_ckv, cache_kpe, page_table):
    S = q_lat.shape[1]
    s = (jnp.einsum('bqhc,bkc->bhqk', q_lat, c_new) + jnp.einsum('bqhr,bkr->bhqk', q_pe, kpe_new)).astype(jnp.float32)
    s = jnp.where(jnp.tril(jnp.ones((S, S), dtype=bool)), s, -jnp.inf)
    m = jnp.max(s, axis=-1)
    p = jnp.exp(s - m[..., None])
    l = jnp.sum(p, axis=-1)
    acc = jnp.einsum('bhqk,bkc->bhqc', p, c_new.astype(jnp.float32))

    def step(carry, pages):
        m, l, acc = carry
        ck = cache_ckv[pages]
        kp = cache_kpe[pages]
        s = (jnp.einsum('bqhc,bkc->bhqk', q_lat, ck) + jnp.einsum('bqhr,bkr->bhqk', q_pe, kp)).astype(jnp.float32)
        m_new = jnp.maximum(m, jnp.max(s, axis=-1))
        corr = jnp.exp(m - m_new)
        p = jnp.exp(s - m_new[..., None])
        l = l * corr + jnp.sum(p, axis=-1)
        acc = acc * corr[..., None] + jnp.einsum('bhqk,bkc->bhqc', p, ck.astype(jnp.float32))
        return (m_new, l, acc), None

    (m, l, acc), _ = lax.scan(step, (m, l, acc), page_table.T)
    o = acc / l[..., None]
    return jnp.transpose(o, (0, 2, 1, 3)).astype(q_lat.dtype)


def run_trunk(x, p_emb, conv0, rnn0, pos, attend, W):
    cos, sin = rope_angles(pos)
    conv_out, rnn_out = [], []
    c_kv, k_pe = None, None
    for i in range(DEPTH):
        if i < N_A_LAYERS:
            j = i
            mix, cs, hs = recurrent_block(x, conv0[j], rnn0[j], W['rg_w_gate'][j], W['rg_w_x'][j], W['rg_conv_w'][j],
                                          W['rg_conv_b'][j], W['rg_w_a'][j], W['rg_b_a'][j], W['rg_w_i'][j],
                                          W['rg_b_i'][j], W['rg_lambda'][j], W['rg_w_out'][j])
            conv_out.append(cs)
            rnn_out.append(hs)
        else:
            j = i - N_A_LAYERS
            q_lat, q_pe = mla_queries(x, W['mla_w_q_a'][j], W['mla_q_norm_g'][j], W['mla_w_q_b'][j], W['kv_w_uk'], cos, sin)
            o_lat = attend(q_lat, q_pe, c_kv, k_pe)
            mix = mla_output(o_lat, W['kv_w_uv'], W['mla_w_o'][j])
        x = layer_norm(DEEPNORM_ALPHA * x + mix, W['ln_mix_g'][i], W['ln_mix_b'][i])
        k = i // 2
        if i % 2 == 0:
            ff = swiglu(x, W['ffn_w_gate'][k], W['ffn_w_up'][k], W['ffn_w_down'][k])
        else:
            ff = moe_swiglu(x, W['moe_w_router'][k], W['moe_w_gate'][k], W['moe_w_up'][k], W['moe_w_down'][k])
        x = layer_norm(DEEPNORM_ALPHA * x + ff, W['ln_ffn_g'][i], W['ln_ffn_b'][i])
        x = x + jax.nn.sigmoid(x @ W['ple_w_gate'][i]) * (p_emb[i] @ W['ple_w_proj'][i])
        if i == N_A_LAYERS - 1:
            c_kv, k_pe = mla_shared_kv(x, W['kv_w_a'], W['kv_norm_g'], cos, sin)
    return x, jnp.stack(conv_out), jnp.stack(rnn_out), c_kv, k_pe


def setup_inputs(seed: int = 0) -> dict:
    key = jax.random.key(seed)
    keys = iter(jax.random.split(key, 64))

    def nrm(shape, scale):
        return scale * jax.random.normal(next(keys), shape, jnp.float32)

    n_pages = PAST_LEN // PAGE_SIZE
    n_phys = (DEC_BATCH * n_pages * 5) // 4
    page_table = jax.random.permutation(next(keys), n_phys)[:DEC_BATCH * n_pages].reshape(DEC_BATCH, n_pages).astype(jnp.int32)
    a8 = jax.random.uniform(next(keys), (N_A_LAYERS, D_RNN), jnp.float32, minval=0.9, maxval=0.999)
    a_base = a8 ** (1.0 / LRU_C)
    rg_lambda = jnp.log(a_base) - jnp.log1p(-a_base)
    d = D_MODEL
    return {
        'x_prompt': nrm((BATCH, SEQ, d), 1.0),
        'x_sample': nrm((DEC_BATCH, DEC_SEQ, d), 1.0),
        'p_prompt': nrm((DEPTH, BATCH, SEQ, D_PLE), 1.0),
        'p_sample': nrm((DEPTH, DEC_BATCH, DEC_SEQ, D_PLE), 1.0),
        'state_conv': nrm((N_A_LAYERS, DEC_BATCH, CONV_WIDTH - 1, D_RNN), 1.0),
        'state_rnn': nrm((N_A_LAYERS, DEC_BATCH, D_RNN), 0.5),
        'cache_ckv': nrm((n_phys, PAGE_SIZE, KV_LORA_RANK), 1.0),
        'cache_kpe': nrm((n_phys, PAGE_SIZE, QK_ROPE_DIM), 1.0),
        'page_table': page_table,
        'ln_mix_g': 1.0 + nrm((DEPTH, d), 0.02),
        'ln_mix_b': nrm((DEPTH, d), 0.02),
        'ln_ffn_g': 1.0 + nrm((DEPTH, d), 0.02),
        'ln_ffn_b': nrm((DEPTH, d), 0.02),
        'rg_w_gate': nrm((N_A_LAYERS, d, D_RNN), d ** -0.5),
        'rg_w_x': nrm((N_A_LAYERS, d, D_RNN), d ** -0.5),
        'rg_conv_w': nrm((N_A_LAYERS, CONV_WIDTH, D_RNN), CONV_WIDTH ** -0.5),
        'rg_conv_b': nrm((N_A_LAYERS, D_RNN), 0.02),
        'rg_w_a': nrm((N_A_LAYERS, N_RNN_BLOCKS, RNN_BLOCK, RNN_BLOCK), RNN_BLOCK ** -0.5),
        'rg_b_a': nrm((N_A_LAYERS, D_RNN), 0.1),
        'rg_w_i': nrm((N_A_LAYERS, N_RNN_BLOCKS, RNN_BLOCK, RNN_BLOCK), RNN_BLOCK ** -0.5),
        'rg_b_i': nrm((N_A_LAYERS, D_RNN), 0.1),
        'rg_lambda': rg_lambda,
        'rg_w_out': nrm((N_A_LAYERS, D_RNN, d), DEEPNORM_BETA * D_RNN ** -0.5),
        'mla_w_q_a': nrm((N_B_LAYERS, d, Q_LORA_RANK), d ** -0.5),
        'mla_q_norm_g': 1.0 + nrm((N_B_LAYERS, Q_LORA_RANK), 0.02),
        'mla_w_q_b': nrm((N_B_LAYERS, Q_LORA_RANK, N_HEADS * (QK_NOPE_DIM + QK_ROPE_DIM)), Q_LORA_RANK ** -0.5),
        'mla_w_o': nrm((N_B_LAYERS, N_HEADS * V_HEAD_DIM, d), DEEPNORM_BETA * (N_HEADS * V_HEAD_DIM) ** -0.5),
        'kv_w_a': nrm((d, KV_LORA_RANK + QK_ROPE_DIM), d ** -0.5),
        'kv_norm_g': 1.0 + nrm((KV_LORA_RANK,), 0.02),
        'kv_w_uk': nrm((KV_LORA_RANK, N_HEADS, QK_NOPE_DIM), KV_LORA_RANK ** -0.5),
        'kv_w_uv': nrm((KV_LORA_RANK, N_HEADS, V_HEAD_DIM), KV_LORA_RANK ** -0.5),
        'ffn_w_gate': nrm((N_DENSE_LAYERS, d, D_FF), d ** -0.5),
        'ffn_w_up': nrm((N_DENSE_LAYERS, d, D_FF), d ** -0.5),
        'ffn_w_down': nrm((N_DENSE_LAYERS, D_FF, d), DEEPNORM_BETA * D_FF ** -0.5),
        'moe_w_router': nrm((N_MOE_LAYERS, d, N_EXPERTS), d ** -0.5),
        'moe_w_gate': nrm((N_MOE_LAYERS, N_EXPERTS, d, D_FF_EXPERT), d ** -0.5),
        'moe_w_up': nrm((N_MOE_LAYERS, N_EXPERTS, d, D_FF_EXPERT), d ** -0.5),
        'moe_w_down': nrm((N_MOE_LAYERS, N_EXPERTS, D_FF_EXPERT, d), DEEPNORM_BETA * D_FF_EXPERT ** -0.5),
        'ple_w_gate': nrm((DEPTH, d, d), d ** -0.5),
        'ple_w_proj': nrm((DEPTH, D_PLE, d), D_PLE ** -0.5),
    }


def reference(x_prompt, x_sample, p_prompt, p_sample, state_conv, state_rnn, cache_ckv, cache_kpe, page_table,
              ln_mix_g, ln_mix_b, ln_ffn_g, ln_ffn_b,
              rg_w_gate, rg_w_x, rg_conv_w, rg_conv_b, rg_w_a, rg_b_a, rg_w_i, rg_b_i, rg_lambda, rg_w_out,
              mla_w_q_a, mla_q_norm_g, mla_w_q_b, mla_w_o,
              kv_w_a, kv_norm_g, kv_w_uk, kv_w_uv,
              ffn_w_gate, ffn_w_up, ffn_w_down,
              moe_w_router, moe_w_gate, moe_w_up, moe_w_down,
              ple_w_gate, ple_w_proj):
    W = {
        'ln_mix_g': ln_mix_g, 'ln_mix_b': ln_mix_b, 'ln_ffn_g': ln_ffn_g, 'ln_ffn_b': ln_ffn_b,
        'rg_w_gate': rg_w_gate, 'rg_w_x': rg_w_x, 'rg_conv_w': rg_conv_w, 'rg_conv_b': rg_conv_b,
        'rg_w_a': rg_w_a, 'rg_b_a': rg_b_a, 'rg_w_i': rg_w_i, 'rg_b_i': rg_b_i, 'rg_lambda': rg_lambda,
        'rg_w_out': rg_w_out,
        'mla_w_q_a': mla_w_q_a, 'mla_q_norm_g': mla_q_norm_g, 'mla_w_q_b': mla_w_q_b, 'mla_w_o': mla_w_o,
        'kv_w_a': kv_w_a, 'kv_norm_g': kv_norm_g, 'kv_w_uk': kv_w_uk, 'kv_w_uv': kv_w_uv,
        'ffn_w_gate': ffn_w_gate, 'ffn_w_up': ffn_w_up, 'ffn_w_down': ffn_w_down,
        'moe_w_router': moe_w_router, 'moe_w_gate': moe_w_gate, 'moe_w_up': moe_w_up, 'moe_w_down': moe_w_down,
        'ple_w_gate': ple_w_gate, 'ple_w_proj': ple_w_proj,
    }
    bsz, seq = x_prompt.shape[0], x_prompt.shape[1]
    dec_s = x_sample.shape[1]
    past_len = page_table.shape[1] * cache_ckv.shape[1]
    conv0_p = jnp.zeros((N_A_LAYERS, bsz, CONV_WIDTH - 1, D_RNN), state_conv.dtype)
    rnn0_p = jnp.zeros((N_A_LAYERS, bsz, D_RNN), state_rnn.dtype)
    y_prompt, conv_p, rnn_p, ckv_p, kpe_p = run_trunk(
        x_prompt, p_prompt, conv0_p, rnn0_p, jnp.arange(seq, dtype=jnp.float32), latent_attention_prompt, W)

    def attend_sample(q_lat, q_pe, c_new, kpe_new):
        return latent_attention_sample(q_lat, q_pe, c_new, kpe_new, cache_ckv, cache_kpe, page_table)

    y_sample, conv_s, rnn_s, ckv_s, kpe_s = run_trunk(
        x_sample, p_sample, state_conv, state_rnn, past_len + jnp.arange(dec_s, dtype=jnp.float32), attend_sample, W)
    return (y_prompt, y_sample, conv_p, rnn_p, ckv_p, kpe_p, conv_s, rnn_s, ckv_s, kpe_s)
```

```python
import contextlib
import numpy as np
import ml_dtypes
import concourse.bass as bass
import concourse.mybir as mybir
from concourse.bass_utils import run_bass_kernel_spmd

F32 = mybir.dt.float32
BF16 = mybir.dt.bfloat16
I32 = mybir.dt.int32
AF = mybir.ActivationFunctionType
ALU = mybir.AluOpType
AX = mybir.AxisListType
NCORES = 8
NEG = -30000.0


def full_cfg():
    return dict(D=1024, SEQ=2048, NSEQ=2, DB=16, DS=8, DPLE=256, DRNN=1536, H=16, QL=384, KVL=256, NOPE=64,
                ROPE=32, VH=64, DFF=2816, NE=8, DFFE=3584, NPG=128, PAGE=128, NPHYS=20480, TT=256, CHB=8,
                PAST=16384, DEPTH=2, THETA=10000.0, DBG=False, MOECC=True, NS=6)


class Op:
    __slots__ = ("eng", "fn", "deps", "sig", "sidx", "dma", "dsem", "dval", "epoch", "bar", "cc")


class Sched:
    ENG = ("pe", "act", "dve", "pool", "sp")

    def __init__(self):
        self.ops = {e: [] for e in self.ENG}
        self.lw = {}
        self.rd = {}
        self.epoch = 0

    def add(self, eng, fn, r=(), w=(), dma=False, cc=False):
        op = Op()
        op.eng, op.fn, op.dma, op.sig, op.sidx, op.epoch, op.bar, op.cc = eng, fn, dma, False, 0, self.epoch, False, cc
        op.dsem = op.dval = None
        deps, seen = [], set()

        def adddep(d):
            if d is not None and id(d) not in seen and d is not op:
                seen.add(id(d))
                deps.append(d)

        for x in r:
            adddep(self.lw.get(x))
        for x in w:
            adddep(self.lw.get(x))
            ent = self.rd.get(x)
            if ent:
                for y in ent[0].values():
                    adddep(y)
                for y in ent[1]:
                    adddep(y)
        for x in r:
            ent = self.rd.setdefault(x, ({}, []))
            if dma or cc:
                ent[1].append(op)
            else:
                ent[0][eng] = op
        for x in w:
            self.lw[x] = op
            self.rd[x] = ({}, [])
        op.deps = deps
        self.ops[eng].append(op)
        return op

    def barrier(self):
        for e in self.ENG:
            op = Op()
            op.eng, op.fn, op.dma, op.sig, op.sidx, op.epoch, op.bar, op.cc = e, None, False, False, 0, self.epoch, True, False
            op.deps = []
            op.dsem = op.dval = None
            self.ops[e].append(op)
        self.epoch += 1
        self.lw = {}
        self.rd = {}

    def finalize(self, nring=8):
        self.barrier()
        nep = self.epoch
        for e in self.ENG:
            for op in self.ops[e]:
                if op.bar:
                    continue
                for d in op.deps:
                    if d.dma or d.cc:
                        continue
                    if d.eng == "pe" and e == "pe":
                        continue
                    d.sig = True
        self.final = {e: [0] * nep for e in self.ENG}
        for e in self.ENG:
            last = None
            for op in self.ops[e]:
                if op.bar:
                    if last is not None:
                        last.sig = True
                    last = None
                elif not op.dma and not op.cc:
                    last = op
            cnt = 0
            for op in self.ops[e]:
                if op.bar:
                    self.final[e][op.epoch] = cnt
                    cnt = 0
                elif op.sig and not op.dma and not op.cc:
                    cnt += 1
                    op.sidx = cnt
        self.nring = nring
        self.ringfinal = {}
        self.ccfinal = [0] * nep
        cccnt = 0
        for e in self.ENG:
            vals = [0] * nring
            i = 0
            for op in self.ops[e]:
                if op.bar:
                    for s in range(nring):
                        self.ringfinal.setdefault((e, s), [0] * nep)[op.epoch] = vals[s]
                    if e == "pool":
                        self.ccfinal[op.epoch] = cccnt
                elif op.dma:
                    s = i % nring
                    i += 1
                    vals[s] += 16
                    op.dsem = (e, s)
                    op.dval = vals[s]
                elif op.cc:
                    cccnt += 1
                    op.dsem = "cc"
                    op.dval = cccnt
        self.nep = nep

    def emit(self, nc, es):
        nep = self.nep
        engsem = {e: [es.enter_context(nc.semaphore(f"s_{e}_{k}")) for k in range(nep)] for e in self.ENG}
        ringsem = {}
        for (e, s) in self.ringfinal:
            if max(self.ringfinal[(e, s)]) > 0:
                ringsem[(e, s)] = es.enter_context(nc.semaphore(f"d_{e}_{s}"))
        ccsem = es.enter_context(nc.semaphore("ccsem"))
        block = es.enter_context(nc.Block())
        sched = self

        def run(ename, eh):
            known = {}
            kd = {}
            kcc = [0]
            for op in sched.ops[ename]:
                if op.bar:
                    ep = op.epoch
                    for f in sched.ENG:
                        v = sched.final[f][ep]
                        if v > 0 and known.get(f, 0) < v:
                            eh.wait_ge(engsem[f][ep], v)
                    for key, sem in ringsem.items():
                        v = sched.ringfinal[key][ep]
                        if v > 0 and kd.get(key, 0) < v:
                            eh.wait_ge(sem, v)
                            kd[key] = v
                    v = sched.ccfinal[ep]
                    if v > kcc[0]:
                        eh.wait_ge(ccsem, v)
                        kcc[0] = v
                    known = {}
                    continue
                ep = op.epoch
                for d in op.deps:
                    if d.cc:
                        if kcc[0] < d.dval:
                            eh.wait_ge(ccsem, d.dval)
                            kcc[0] = d.dval
                    elif d.dma:
                        if kd.get(d.dsem, 0) < d.dval:
                            eh.wait_ge(ringsem[d.dsem], d.dval)
                            kd[d.dsem] = d.dval
                    elif d.eng == "pe" and ename == "pe":
                        continue
                    else:
                        if known.get(d.eng, 0) < d.sidx:
                            eh.wait_ge(engsem[d.eng][ep], d.sidx)
                            known[d.eng] = d.sidx
                if op.dma:
                    pv = op.dval - 16
                    if pv > 0 and kd.get(op.dsem, 0) < pv:
                        eh.wait_ge(ringsem[op.dsem], pv)
                        kd[op.dsem] = pv
                ins = op.fn(eh)
                if op.dma:
                    ins.then_inc(ringsem[op.dsem], 16)
                elif op.cc:
                    ins.then_inc(ccsem)
                elif op.sig:
                    ins.then_inc(engsem[ename][ep], 1)

        @block.tensor
        def _(eh):
            run("pe", eh)

        @block.scalar
        def _(eh):
            run("act", eh)

        @block.vector
        def _(eh):
            run("dve", eh)

        @block.gpsimd
        def _(eh):
            run("pool", eh)

        @block.sync
        def _(eh):
            run("sp", eh)


def kmaj(w):
    K, N = w.shape
    return np.ascontiguousarray(w.reshape(K // 128, 128, N).transpose(1, 0, 2))


def colv(v):
    return np.ascontiguousarray(v.reshape(-1, 128).T)


def rope_pad_cols(w, swap):
    K = w.shape[0]
    o = np.zeros((K, 64), np.float32)
    a, b = (w[:, 16:32], w[:, 0:16]) if swap else (w[:, 0:16], w[:, 16:32])
    o[:, 0:16] = a
    o[:, 32:48] = b
    return o


class Builder:
    def __init__(self, cfg):
        self.c = cfg
        self.nc = bass.Bass("TRN2", target_bir_lowering=False)
        self.S = Sched()
        self.din = {}
        self.dout = {}
        self.bank_i = 0

    def inp(self, name, shape, dt=F32):
        t = self.nc.dram_tensor(name, list(shape), dt, kind="ExternalInput").ap()
        self.din[name] = (tuple(shape), dt)
        return t

    def outp(self, name, shape, dt=F32):
        t = self.nc.dram_tensor(name, list(shape), dt, kind="ExternalOutput").ap()
        self.dout[name] = (tuple(shape), dt)
        return t

    def scratch(self, name, shape, dt, dbg=False, shared=False):
        if dbg and self.c["DBG"]:
            return self.outp(name, shape, dt)
        if shared:
            return self.nc.dram_tensor(name, list(shape), dt, addr_space="Shared").ap()
        return self.nc.dram_tensor(name, list(shape), dt).ap()

    def sb_reset(self, keep=0):
        self.sb_off = keep

    def T(self, shape, dt=F32):
        n = int(np.prod(shape))
        esz = 4 if dt in (F32, I32) else 2
        nbytes = (n * esz + 31) // 32 * 32
        off = self.sb_off
        self.sb_off += nbytes
        assert self.sb_off <= self.SBW * 4, f"SBUF overflow {self.sb_off}"
        ap = self.sb[:, off // 4:(off + nbytes) // 4]
        if esz == 2:
            ap = ap.bitcast(dt)
        elif dt == I32:
            ap = ap.bitcast(I32)
        ap = ap[:, 0:n]
        if len(shape) == 2:
            ap = ap.rearrange("p (a b) -> p a b", a=shape[0])
        elif len(shape) == 3:
            ap = ap.rearrange("p (a b c) -> p a b c", a=shape[0], b=shape[1])
        return ap

    def bcreg(self, e, val):
        if getattr(self, '_bcreg', None) is None:
            self._bcreg = e.to_reg(val)
        return self._bcreg

    def bank(self):
        b = self.bank_i % 8
        self.bank_i += 1
        return b

    def psf(self, b, n=512):
        return self.ps[:, b * 512:b * 512 + n]

    def psb(self, b, n=1024):
        return self.ps[:, b * 512:(b + 1) * 512].bitcast(BF16)[:, 0:n]

    def pe(self, fn, r, w):
        return self.S.add("pe", fn, r, w)

    def act(self, fn, r, w):
        return self.S.add("act", fn, r, w)

    def dve(self, fn, r, w):
        return self.S.add("dve", fn, r, w)

    def pool(self, fn, r, w):
        return self.S.add("pool", fn, r, w)

    def dma(self, out, in_, r, w, q="sp"):
        return self.S.add(q, lambda e: e.dma_start(out=out, in_=in_), r, w, dma=True)

    def mm(self, ps_ap, pairs, r, w):
        n = len(pairs)
        for i, (l, rr) in enumerate(pairs):
            self.pe(lambda e, l=l, rr=rr, i=i: e.matmul(ps_ap, lhsT=l, rhs=rr, start=(i == 0), stop=(i == n - 1)), r, w)

    def ln_stats(self, zs, N, eps, D, tag, rms=False, npart=128):
        b1 = self.bank()
        sq = self.tmp_sq
        if not rms:
            b0 = self.bank()
            self.mm(self.psf(b0, N), [(self.ones[:, :], z) for z in zs], [tag + "z"], [("ps", b0)])
        for i, z in enumerate(zs):
            self.act(lambda e, z=z, i=i: e.activation(out=sq[:, i, 0:N], in_=z, func=AF.Square), [tag + "z"], [("sq", i)])
        self.mm(self.psf(b1, N), [(self.ones[:, :], sq[:, i, 0:N]) for i in range(len(zs))],
                [("sq", i) for i in range(len(zs))], [("ps", b1)])
        mean, var, rstd = self.st_mean[:, 0:N], self.st_var[:, 0:N], self.st_rstd[:, 0:N]
        if not rms:
            self.act(lambda e: e.activation(out=mean, in_=self.psf(b0, N), func=AF.Identity, scale=1.0 / D), [("ps", b0)], ["st_mean"])
            self.dve(lambda e: e.tensor_tensor(out=var, in0=mean, in1=mean, op=ALU.mult), ["st_mean"], ["st_var"])
            self.dve(lambda e: e.scalar_tensor_tensor(out=var, in0=self.psf(b1, N), scalar=1.0 / D, in1=var, op0=ALU.mult, op1=ALU.subtract),
                     [("ps", b1), "st_var"], ["st_var"])
            self.act(lambda e: e.activation(out=var, in_=var, func=AF.Sqrt, bias=self.epsc[:, 0:1], scale=1.0), ["st_var"], ["st_var"])
        else:
            self.act(lambda e: e.activation(out=var, in_=self.psf(b1, N), func=AF.Sqrt, bias=self.epsc[:, 1:2], scale=1.0 / D), [("ps", b1)], ["st_var"])
        self.dve(lambda e: e.reciprocal(out=rstd, in_=var), ["st_var"], ["st_rstd"])
        return (None if rms else mean), rstd

    def layer_norm(self, zf, KD, N, gcol, bcol, outf, outb, tag):
        c = self.c
        mean, rstd = self.ln_stats([zf[:, k, 0:N] for k in range(KD)], N, 1e-5, c["D"], tag)
        for k in range(KD):
            t = self.tmp_ln[:, k % 2, 0:N]
            self.dve(lambda e, k=k, t=t: e.tensor_tensor(out=t, in0=zf[:, k, 0:N], in1=mean, op=ALU.subtract), [tag + "z", "st_mean"], [("lnt", k % 2)])
            self.dve(lambda e, k=k, t=t: e.tensor_tensor(out=t, in0=t, in1=rstd, op=ALU.mult), ["st_rstd", ("lnt", k % 2)], [("lnt", k % 2)])
            self.act(lambda e, k=k, t=t: e.activation(out=outf[:, k, 0:N], in_=t, func=AF.Identity, bias=bcol[:, k:k + 1], scale=gcol[:, k:k + 1]),
                     [("lnt", k % 2)], [tag + "of"])
            if outb is not None:
                self.pool(lambda e, k=k: e.tensor_copy(out=outb[:, k, 0:N], in_=outf[:, k, 0:N]), [tag + "of"], [tag + "ob"])

    def build(self):
        c = self.c
        nc = self.nc
        D, SEQ, NSEQ, DB, DS = c["D"], c["SEQ"], c["NSEQ"], c["DB"], c["DS"]
        KD = D // 128
        TP = NSEQ * SEQ
        TS = DB * DS
        assert TS == 128
        T = TP + TS
        TT = c["TT"]
        DRNN = c["DRNN"]
        NRC = DRNN // 128
        DFF = c["DFF"]
        NFC = DFF // 128
        DPLE = c["DPLE"]
        NPC = DPLE // 128
        H, QL = c["H"], c["QL"]
        QLC = QL // 128
        NE, DFFE = c["NE"], c["DFFE"]
        NG = DFFE // 512
        NPG, PAGE, NPHYS, CHB = c["NPG"], c["PAGE"], c["NPHYS"], c["CHB"]
        PR = NPHYS // NCORES
        CP = 1 << CHB
        NCH = PR // CP
        assert NCH * CP == PR and PAGE == 128
        ALPHA = (2.0 * c["DEPTH"]) ** 0.25
        SCALE = (c["NOPE"] + c["ROPE"]) ** -0.5
        self.KD, self.Ttok = KD, T
        S = self.S
        tiles = []
        for s in range(NSEQ):
            for t0 in range(0, SEQ, TT):
                tiles.append(("p", s, s * SEQ + t0, TT, t0 == 0, t0 + TT == SEQ))
        tiles.append(("s", 0, TP, TS, True, True))
        NT128 = T // 128

        xT = self.inp("xT", [128, KD, T])
        pT = self.inp("pT", [2, 128, NPC, T])
        cst = self.inp("cst", [128, NRC, DB, 3])
        hst = self.inp("hst", [128, NRC, DB])
        ckv_sh = self.inp("ckv_sh", [PR * 128, 256])
        kpe_sh = self.inp("kpe_sh", [PR * 128, 32])
        ptT = self.inp("ptT", [128, DB], I32)
        cosT = self.inp("cosT", [64, T])
        sinT = self.inp("sinT", [64, T])
        maskp = self.inp("maskp", [128, 248], BF16)
        masks = self.inp("masks", [128, 256], BF16)
        identb = self.inp("identb", [128, 128], BF16)
        identf = self.inp("identf", [128, 128])
        vec_names = []

        def vec(name, n):
            vec_names.append((name, n))

        vec("ln_mix_g0", KD), vec("ln_mix_b0", KD), vec("ln_ffn_g0", KD), vec("ln_ffn_b0", KD)
        vec("ln_mix_g1", KD), vec("ln_mix_b1", KD), vec("ln_ffn_g1", KD), vec("ln_ffn_b1", KD)
        vec("conv_w", NRC * 4), vec("conv_b", NRC), vec("b_a", NRC), vec("b_i", NRC), vec("lam", NRC)
        vec("kvg", 2), vec("qg", QLC)
        voff = {}
        o = 0
        for n_, k_ in vec_names:
            voff[n_] = o
            o += k_
        NV = o
        self.vec_names, self.voff, self.NV = vec_names, voff, NV
        vecs = self.inp("vecs", [128, NV])
        w_rg_g = self.inp("w_rg_g", [128, KD, DRNN])
        w_rg_x = self.inp("w_rg_x", [128, KD, DRNN])
        w_rg_a = self.inp("w_rg_a", [128, NRC, 128])
        w_rg_i = self.inp("w_rg_i", [128, NRC, 128])
        w_rg_o = self.inp("w_rg_o", [128, NRC, D])
        w_f_g = self.inp("w_f_g", [128, KD, DFF])
        w_f_u = self.inp("w_f_u", [128, KD, DFF])
        w_f_d = self.inp("w_f_d", [128, NFC, D])
        w_pg = self.inp("w_pg", [2, 128, KD, D])
        w_pp = self.inp("w_pp", [2, 128, NPC, D])
        w_kvc = self.inp("w_kvc", [128, KD, 256])
        w_kvA = self.inp("w_kvA", [128, KD, 64])
        w_kvB = self.inp("w_kvB", [128, KD, 64])
        w_qa = self.inp("w_qa", [128, KD, QL])
        w_qbN = self.inp("w_qbN", [128, QLC, H * 64])
        w_qbA = self.inp("w_qbA", [128, QLC, H * 64])
        w_qbB = self.inp("w_qbB", [128, QLC, H * 64])
        w_ukT = self.inp("w_ukT", [128, H // 2, 256])
        w_uvP = self.inp("w_uvP", [128, 2, H, 128])
        w_o = self.inp("w_o", [128, H * 64 // 128, D])
        w_r = self.inp("w_r", [128, KD, NE])
        GW = KD * 512
        DWG = 4 * D
        EROW = NG * (2 * GW + DWG)

        yT = self.outp("yT", [128, KD, T])
        conv_p = self.outp("conv_p", [128, NRC, NSEQ, 3])
        rnn_p = self.outp("rnn_p", [128, NSEQ, NRC])
        conv_s = self.outp("conv_s", [128, NRC, DB, 3])
        rnn_s = self.outp("rnn_s", [128, NRC, DB])
        ckvT = self.outp("ckvT", [128, 2, T])
        kpeT = self.outp("kpeT", [64, T])

        x1f_d = self.scratch("x1f_d", [128, KD, T], F32, dbg=True)
        x1b_d = self.scratch("x1b_d", [128, KD, T], BF16)
        hT_d = self.scratch("hT_d", [128, NFC, T], BF16)
        x3f_d = self.scratch("x3f_d", [128, KD, T], F32, dbg=True)
        x3b_d = self.scratch("x3b_d", [128, KD, T], BF16)
        CT_d = self.scratch("CT_d", [128, 2, T], BF16)
        KP_d = self.scratch("KP_d", [64, T], BF16)
        Ctok_d = self.scratch("Ctok_d", [128, NT128, 256], BF16)
        x4f_d = self.scratch("x4f_d", [128, KD, T], F32, dbg=True)
        x4b_d = self.scratch("x4b_d", [128, KD, T], BF16)
        gates_d = self.scratch("gates_d", [128, NT128, NE], F32, dbg=True)
        moeT_d = self.scratch("moeT_d", [128, KD, T], F32, dbg=True)
        moe_full = self.scratch("moe_full", [NCORES * 128, EROW], BF16, shared=bool(c.get("MOECC")))
        NS = c.get('NS', 6)
        TSLS = [24, 24, 24, 24, 16, 16] if NS == 6 else [128 // NS] * NS
        TOFF = [sum(TSLS[:k]) for k in range(NS)]
        self.TSLS, self.TOFF, self.NS = TSLS, TOFF, NS
        cb = [self.scratch(f"cb{k}", [PR, TSLS[k] * 256], BF16) for k in range(NS)]
        cfull = [self.scratch(f"cfull{k}", [NCORES * PR, TSLS[k] * 256], BF16, shared=not c.get("NOCC")) for k in range(NS)]
        kb = self.scratch("kb", [PR, 128 * 32], BF16)
        kfull = self.scratch("kfull", [NCORES * PR, 128 * 32], BF16, shared=not c.get("NOCC"))

        es = contextlib.ExitStack()
        self.es = es
        self.SBW = 52224
        self.sb = es.enter_context(nc.sbuf_tensor("sb", [128, self.SBW], F32))
        self.ps = es.enter_context(nc.psum_tensor("ps", [128, 4096], F32))

        self.sb_reset(0)
        self.vecs = self.T([1, NV])[:, 0, :]
        self.ones = self.T([1, 128])[:, 0, :]
        self.identb = self.T([1, 128], BF16)[:, 0, :]
        self.identf = self.T([1, 128])[:, 0, :]
        self.epsc = self.T([1, 2])[:, 0, :]
        self.nsp8 = self.T([1, NRC])[:, 0, :]
        self.nsp16 = self.T([1, NRC])[:, 0, :]
        self.st_mean = self.T([1, TT])[:, 0, :]
        self.st_var = self.T([1, TT])[:, 0, :]
        self.st_rstd = self.T([1, TT])[:, 0, :]
        self.tmp_ln = self.T([2, TT])
        self.tmp_sq = self.T([KD, TT])
        spt = self.T([4, NRC])
        KEEP = self.sb_off
        V = self.vecs

        def vcol(name, k=0):
            o_ = voff[name] + k
            return V[:, o_:o_ + 1]

        def vblk(name, n):
            return V[:, voff[name]:voff[name] + n]

        self.dma(V, vecs[:, :], [], ["vecs"])
        self.dma(self.identb, identb[:, :], [], ["identb"])
        self.dma(self.identf, identf[:, :], [], ["identf"])
        self.pool(lambda e: e.memset(self.ones, 1.0), [], ["ones"])
        self.pool(lambda e: e.memset(self.epsc[:, 0:1], 1e-5), [], ["epsc"])
        self.pool(lambda e: e.memset(self.epsc[:, 1:2], 1e-6), [], ["epsc"])
        lamv = vblk("lam", NRC)
        xx, ser, lnv, msk = spt[:, 0, :], spt[:, 1, :], spt[:, 2, :], spt[:, 3, :]
        self.act(lambda e: e.activation(out=xx, in_=lamv, func=AF.Exp, scale=-1.0), ["vecs"], ["sp_x"])
        self.act(lambda e: e.activation(out=lnv, in_=xx, func=AF.Ln, bias=1.0, scale=1.0), ["sp_x"], ["sp_ln"])
        self.dve(lambda e: e.tensor_scalar(out=ser, in0=xx, scalar1=1.0 / 3.0, scalar2=-0.5, op0=ALU.mult, op1=ALU.add), ["sp_x"], ["sp_ser"])
        self.dve(lambda e: e.tensor_tensor(out=ser, in0=ser, in1=xx, op=ALU.mult), ["sp_ser", "sp_x"], ["sp_ser"])
        self.dve(lambda e: e.tensor_scalar(out=ser, in0=ser, scalar1=1.0, scalar2=None, op0=ALU.add), ["sp_ser"], ["sp_ser"])
        self.dve(lambda e: e.tensor_tensor(out=ser, in0=ser, in1=xx, op=ALU.mult), ["sp_ser", "sp_x"], ["sp_ser"])
        self.dve(lambda e: e.tensor_scalar(out=msk, in0=xx, scalar1=0.05, scalar2=None, op0=ALU.is_lt), ["sp_x"], ["sp_m"])
        self.dve(lambda e: e.tensor_tensor(out=ser, in0=ser, in1=lnv, op=ALU.subtract), ["sp_ser", "sp_ln"], ["sp_ser"])
        self.dve(lambda e: e.tensor_tensor(out=ser, in0=ser, in1=msk, op=ALU.mult), ["sp_ser", "sp_m"], ["sp_ser"])
        self.dve(lambda e: e.tensor_tensor(out=ser, in0=ser, in1=lnv, op=ALU.add), ["sp_ser", "sp_ln"], ["sp_ser"])
        self.dve(lambda e: e.tensor_scalar(out=self.nsp8, in0=ser, scalar1=-8.0, scalar2=None, op0=ALU.mult), ["sp_ser"], ["nsp8"])
        self.dve(lambda e: e.tensor_scalar(out=self.nsp16, in0=ser, scalar1=-16.0, scalar2=None, op0=ALU.mult), ["sp_ser"], ["nsp16"])

        def cc(kind, i_ap, o_ap, r, w):
            S.add("pool", lambda e: e.collective_compute(kind, ALU.bypass, replica_groups=[list(range(NCORES))], ins=[i_ap], outs=[o_ap]),
                  r, w, cc=True)

        if c.get("MOECC"):
            moe_sh = self.inp("moe_sh", [128, EROW])
            moe_b = self.scratch("moe_b", [128, EROW], BF16)
            self.dma(moe_b[:, :], moe_sh[:, :], [], ["moe_b"], q="pool")
            cc("AllGather", moe_b[:, :], moe_full[:, :], ["moe_b"], ["moe_full"])
        else:
            moe_all = self.inp("moe_all", [NCORES * 128, EROW])
            self.dma(moe_full[:, :], moe_all[:, :], [], ["moe_full"], q="pool")
        if not c.get("NOCC"):
            self.dma(kb[:, :], kpe_sh.rearrange("(p t) c -> p (t c)", t=128), [], ["kb"], q="pool")
            ck3 = ckv_sh.rearrange("(p t) c -> p t c", t=128)
            for k in range(NS):
                self.dma(cb[k].rearrange("p (t c) -> p t c", c=256), ck3[:, TOFF[k]:TOFF[k] + TSLS[k], :], [], [("cb", k)], q="pool")
            cc("AllGather", kb[:, :], kfull[:, :], ["kb"], ["kfull"])
            for k in range(NS):
                cc("AllGather", cb[k][:, :], cfull[k][:, :], [("cb", k)], [("cfull", k)])
        else:
            ckv_all = self.inp("ckv_all", [NCORES * PR * 128, 256])
            kpe_all = self.inp("kpe_all", [NCORES * PR * 128, 32])
            self.dma(kfull[:, :], kpe_all.rearrange("(p t) c -> p (t c)", t=128), [], ["kfull"], q="pool")
            ck3 = ckv_all.rearrange("(p t) c -> p t c", t=128)
            for k in range(NS):
                self.dma(cfull[k].rearrange("p (t c) -> p t c", c=256), ck3[:, TOFF[k]:TOFF[k] + TSLS[k], :], [], [("cfull", k)], q="pool")
        S.barrier()

        self.phase1(tiles, xT, cst, hst, w_rg_g, w_rg_x, w_rg_a, w_rg_i, w_rg_o, x1f_d, x1b_d, conv_p, rnn_p, conv_s, rnn_s, KEEP, vcol, vblk, ALPHA)
        self.phase2a(tiles, w_f_g, w_f_u, x1b_d, hT_d, KEEP)
        self.phase2b(tiles, w_f_d, w_pg, w_pp, w_kvc, w_kvA, w_kvB, x1f_d, hT_d, pT, cosT, sinT, x3f_d, x3b_d, CT_d, KP_d, Ctok_d, ckvT, kpeT,
                     KEEP, vcol, vblk, ALPHA)
        D_ = dict(w_qa=w_qa, w_qbN=w_qbN, w_qbA=w_qbA, w_qbB=w_qbB, w_ukT=w_ukT, w_uvP=w_uvP, w_o=w_o, w_r=w_r, maskp=maskp, masks=masks,
                  x3f_d=x3f_d, x3b_d=x3b_d, cosT=cosT, sinT=sinT, CT_d=CT_d, KP_d=KP_d, Ctok_d=Ctok_d, x4f_d=x4f_d, x4b_d=x4b_d, gates_d=gates_d,
                  cfull=cfull, kfull=kfull, ptT=ptT)
        if c.get("STOP") == "2b":
            return es
        self.phase4("p", D_, KEEP, vcol, vblk, ALPHA, SCALE)
        if c.get("STOP") != "4p":
            self.phase4("s", D_, KEEP, vcol, vblk, ALPHA, SCALE)
        D_.update(moe_full=moe_full, moeT_d=moeT_d, w_pg=w_pg, w_pp=w_pp, pT=pT, yT=yT)
        self.phase5(D_, KEEP)
        self.phase6(tiles, D_, KEEP, vcol, vblk, ALPHA)
        return es

    def phase1(self, tiles, xT, cst, hst, w_g, w_x, w_a, w_i, w_o, x1f_d, x1b_d, conv_p, rnn_p, conv_s, rnn_s, KEEP, vcol, vblk, ALPHA):
        c = self.c
        D, DRNN, DB, DS, TT = c["D"], c["DRNN"], c["DB"], c["DS"], c["TT"]
        KD, NRC = D // 128, DRNN // 128
        self.sb_reset(KEEP)
        Wg = self.T([KD, DRNN], BF16)
        Wx = self.T([KD, DRNN], BF16)
        Wa = self.T([NRC, 128], BF16)
        Wi = self.T([NRC, 128], BF16)
        Wo = self.T([NRC, D], BF16)
        xf = [self.T([KD, TT]) for _ in range(2)]
        xb = self.T([KD, TT], BF16)
        gate = self.T([NRC, TT])
        uext = self.T([NRC, TT + 3])
        uexs = self.T([NRC, DB, DS + 3])
        hcar = self.T([1, NRC])[:, 0, :]
        h0s = self.T([NRC, DB])
        hfin = self.T([NRC, DB])
        wk = [self.T([6, TT]) for _ in range(2)]
        convb = [self.T([1, TT], BF16)[:, 0, :] for _ in range(2)]
        hg = self.T([NRC, TT], BF16)
        zf = self.T([KD, TT])
        of = self.T([KD, TT])
        ob = self.T([KD, TT], BF16)
        self.dma(Wg, w_g[:, :, :], [], ["Wg"], q="pool")
        self.dma(Wx, w_x[:, :, :], [], ["Wx"], q="pool")
        self.dma(Wa, w_a[:, :, :], [], ["Wa"], q="pool")
        self.dma(Wi, w_i[:, :, :], [], ["Wi"], q="pool")
        self.dma(Wo, w_o[:, :, :], [], ["Wo"], q="pool")
        self.dma(uexs[:, :, :, 0:3], cst[:, :, :, :], [], ["uexs"])
        self.dma(h0s, hst[:, :, :], [], ["h0s"])

        def load(i):
            kind, s, t0, N, first, last = tiles[i]
            self.dma(xf[i % 2][:, :, 0:N], xT[:, :, t0:t0 + N], [], [("xf", i % 2)])

        load(0)
        for i, (kind, s, t0, N, first, last) in enumerate(tiles):
            if i + 1 < len(tiles):
                load(i + 1)
            X = xf[i % 2]
            for k in range(KD):
                self.pool(lambda e, k=k, X=X, N=N: e.tensor_copy(out=xb[:, k, 0:N], in_=X[:, k, 0:N]), [("xf", i % 2)], ["xb"])
            if kind == "p" and first:
                self.pool(lambda e: e.memset(uext[:, :, 0:3], 0.0), [], ["uext"])
            for rc in range(NRC):
                W6 = wk[rc % 2]
                wr = ("wk", rc % 2)
                bg, bu = self.bank(), self.bank()
                self.mm(self.psf(bg, N), [(Wg[:, k, rc * 128:(rc + 1) * 128], xb[:, k, 0:N]) for k in range(KD)], ["Wg", "xb"], [("ps", bg)])
                self.mm(self.psf(bu, N), [(Wx[:, k, rc * 128:(rc + 1) * 128], xb[:, k, 0:N]) for k in range(KD)], ["Wx", "xb"], [("ps", bu)])
                self.act(lambda e, rc=rc, bg=bg, N=N: e.activation(out=gate[:, rc, 0:N], in_=self.psf(bg, N), func=AF.Gelu_apprx_tanh), [("ps", bg)], ["gate"])
                cw = lambda k, rc=rc: vcol("conv_w", rc * 4 + k)
                cv = W6[:, 0, 0:N]
                if kind == "p":
                    self.act(lambda e, rc=rc, bu=bu, N=N: e.activation(out=uext[:, rc, 3:3 + N], in_=self.psf(bu, N), func=AF.Identity), [("ps", bu)], ["uext"])
                    usl = lambda k, rc=rc, N=N: uext[:, rc, k:k + N]
                    cvv = cv
                else:
                    self.act(lambda e, rc=rc, bu=bu, N=N: e.activation(out=uexs[:, rc, :, 3:3 + DS], in_=self.psf(bu, N).rearrange("p (b s) -> p b s", b=DB),
                                                                        func=AF.Identity), [("ps", bu)], ["uexs"])
                    usl = lambda k, rc=rc: uexs[:, rc, :, k:k + DS]
                    cvv = cv.rearrange("p (b s) -> p b s", b=DB)
                ures = "uext" if kind == "p" else "uexs"
                self.dve(lambda e, usl=usl, cw=cw, cvv=cvv, rc=rc: e.tensor_scalar(out=cvv, in0=usl(0), scalar1=cw(0), scalar2=vcol("conv_b", rc), op0=ALU.mult, op1=ALU.add),
                         [ures, "vecs"], [wr])
                for k in range(1, 4):
                    self.dve(lambda e, usl=usl, cw=cw, cvv=cvv, k=k: e.scalar_tensor_tensor(out=cvv, in0=usl(k), scalar=cw(k), in1=cvv, op0=ALU.mult, op1=ALU.add),
                             [ures, "vecs", wr], [wr])
                cvb = convb[rc % 2][:, 0:N]
                self.pool(lambda e, cvb=cvb, cv=cv: e.tensor_copy(out=cvb, in_=cv), [wr], [("cvb", rc % 2)])
                ba, bi = self.bank(), self.bank()
                self.mm(self.psf(ba, N), [(Wa[:, rc, :], cvb)], ["Wa", ("cvb", rc % 2)], [("ps", ba)])
                self.mm(self.psf(bi, N), [(Wi[:, rc, :], cvb)], ["Wi", ("cvb", rc % 2)], [("ps", bi)])
                r_, i_, a_, q_, h_ = (W6[:, j, 0:N] for j in range(1, 6))
                self.act(lambda e, r_=r_, ba=ba, N=N, rc=rc: e.activation(out=r_, in_=self.psf(ba, N), func=AF.Sigmoid, bias=vcol("b_a", rc), scale=1.0), [("ps", ba), "vecs"], [wr])
                self.act(lambda e, i_=i_, bi=bi, N=N, rc=rc: e.activation(out=i_, in_=self.psf(bi, N), func=AF.Sigmoid, bias=vcol("b_i", rc), scale=1.0), [("ps", bi), "vecs"], [wr])
                self.act(lambda e, a_=a_, r_=r_, rc=rc: e.activation(out=a_, in_=r_, func=AF.Exp, scale=self.nsp8[:, rc:rc + 1]), [wr, "nsp8"], [wr])
                self.act(lambda e, q_=q_, r_=r_, rc=rc: e.activation(out=q_, in_=r_, func=AF.Exp, scale=self.nsp16[:, rc:rc + 1]), [wr, "nsp16"], [wr])
                self.act(lambda e, q_=q_: e.activation(out=q_, in_=q_, func=AF.Sqrt, bias=1.0, scale=-1.0), [wr], [wr])
                self.dve(lambda e, i_=i_, cv=cv: e.tensor_tensor(out=i_, in0=i_, in1=cv, op=ALU.mult), [wr], [wr])
                self.dve(lambda e, i_=i_, q_=q_: e.tensor_tensor(out=i_, in0=i_, in1=q_, op=ALU.mult), [wr], [wr])
                if kind == "p":
                    init = 0.0 if first else hcar[:, rc:rc + 1]
                    self.dve(lambda e, h_=h_, a_=a_, i_=i_, init=init: e.tensor_tensor_scan(out=h_, data0=a_, data1=i_, initial=init, op0=ALU.mult, op1=ALU.add),
                             [wr, "hcar"], [wr])
                    self.dve(lambda e, h_=h_, rc=rc, N=N: e.tensor_copy(out=hcar[:, rc:rc + 1], in_=h_[:, N - 1:N]), [wr], ["hcar"])
                else:
                    for b in range(DB):
                        sl = slice(b * DS, (b + 1) * DS)
                        self.dve(lambda e, h_=h_, a_=a_, i_=i_, sl=sl, rc=rc, b=b: e.tensor_tensor_scan(out=h_[:, sl], data0=a_[:, sl], data1=i_[:, sl],
                                                                                                     initial=h0s[:, rc, b:b + 1], op0=ALU.mult, op1=ALU.add),
                                 [wr, "h0s"], [wr])
                    self.dve(lambda e, h_=h_, rc=rc: e.tensor_copy(out=hfin[:, rc, :], in_=h_.rearrange("p (b s) -> p b s", b=DB)[:, :, DS - 1]), [wr], ["hfin"])
                self.dve(lambda e, h_=h_, rc=rc, N=N: e.tensor_tensor(out=hg[:, rc, 0:N], in0=h_, in1=gate[:, rc, 0:N], op=ALU.mult), [wr, "gate"], ["hg"])
            if kind == "p" and last:
                self.dma(conv_p[:, :, s, :], uext[:, :, N:N + 3], ["uext"], [])
                self.dma(rnn_p[:, s, :], hcar, ["hcar"], [])
            if kind == "s":
                self.dma(conv_s[:, :, :, :], uexs[:, :, :, DS:DS + 3], ["uexs"], [])
                self.dma(rnn_s[:, :, :], hfin, ["hfin"], [])
            if kind == "p" and not last:
                self.pool(lambda e, N=N: e.tensor_copy(out=uext[:, :, 0:3], in_=uext[:, :, N:N + 3]), ["uext"], ["uext"])
            for oc in range(KD):
                b = self.bank()
                self.mm(self.psf(b, N), [(Wo[:, rc, oc * 128:(oc + 1) * 128], hg[:, rc, 0:N]) for rc in range(NRC)], ["Wo", "hg"], [("ps", b)])
                self.dve(lambda e, oc=oc, b=b, X=X, N=N: e.scalar_tensor_tensor(out=zf[:, oc, 0:N], in0=X[:, oc, 0:N], scalar=ALPHA, in1=self.psf(b, N),
                                                                             op0=ALU.mult, op1=ALU.add), [("ps", b), ("xf", i % 2)], ["l0z"])
            self.layer_norm(zf, KD, N, vblk("ln_mix_g0", KD), vblk("ln_mix_b0", KD), of, ob, "l0")
            self.dma(x1f_d[:, :, t0:t0 + N], of[:, :, 0:N], ["l0of"], [])
            self.dma(x1b_d[:, :, t0:t0 + N], ob[:, :, 0:N], ["l0ob"], [])
        self.S.barrier()

    def phase2a(self, tiles, w_g, w_u, x1b_d, hT_d, KEEP):
        c = self.c
        D, DFF, TT = c["D"], c["DFF"], c["TT"]
        KD, NFC = D // 128, DFF // 128
        self.sb_reset(KEEP)
        Wg = self.T([KD, DFF], BF16)
        Wu = self.T([KD, DFF], BF16)
        xb = [self.T([KD, TT], BF16) for _ in range(2)]
        hT = [self.T([NFC, TT], BF16) for _ in range(2)]
        sg = [self.T([1, TT])[:, 0, :] for _ in range(2)]
        self.dma(Wg, w_g[:, :, :], [], ["Wg"], q="pool")
        self.dma(Wu, w_u[:, :, :], [], ["Wu"], q="pool")

        def load(i):
            kind, s, t0, N, first, last = tiles[i]
            self.dma(xb[i % 2][:, :, 0:N], x1b_d[:, :, t0:t0 + N], [], [("xb", i % 2)])

        load(0)
        for i, (kind, s, t0, N, first, last) in enumerate(tiles):
            if i + 1 < len(tiles):
                load(i + 1)
            X, Hh = xb[i % 2], hT[i % 2]
            for fc in range(NFC):
                bg, bu = self.bank(), self.bank()
                self.mm(self.psf(bg, N), [(Wg[:, k, fc * 128:(fc + 1) * 128], X[:, k, 0:N]) for k in range(KD)], ["Wg", ("xb", i % 2)], [("ps", bg)])
                self.mm(self.psf(bu, N), [(Wu[:, k, fc * 128:(fc + 1) * 128], X[:, k, 0:N]) for k in range(KD)], ["Wu", ("xb", i % 2)], [("ps", bu)])
                sgt = sg[fc % 2][:, 0:N]
                self.act(lambda e, sgt=sgt, bg=bg, N=N: e.activation(out=sgt, in_=self.psf(bg, N), func=AF.Silu), [("ps", bg)], [("sg", fc % 2)])
                self.dve(lambda e, sgt=sgt, bu=bu, N=N, fc=fc, Hh=Hh: e.tensor_tensor(out=Hh[:, fc, 0:N], in0=sgt, in1=self.psf(bu, N), op=ALU.mult),
                         [("sg", fc % 2), ("ps", bu)], [("hT", i % 2)])
            self.dma(hT_d[:, :, t0:t0 + N], Hh[:, :, 0:N], [("hT", i % 2)], [])
        self.S.barrier()

    def ple(self, layer, x2f, x2b, pb, Wpg, Wpp, KD, NPC, N, outf, outb, tagr, tagw):
        for oc in range(KD):
            bg, bp = self.bank(), self.bank()
            self.mm(self.psf(bg, N), [(Wpg[:, k, oc * 128:(oc + 1) * 128], x2b[:, k, 0:N]) for k in range(KD)], ["Wpg", tagr + "ob"], [("ps", bg)])
            self.mm(self.psf(bp, N), [(Wpp[:, k, oc * 128:(oc + 1) * 128], pb[:, k, 0:N]) for k in range(NPC)], ["Wpp", "pb"], [("ps", bp)])
            t = self.tmp_ln[:, oc % 2, 0:N]
            self.act(lambda e, t=t, bg=bg, N=N: e.activation(out=t, in_=self.psf(bg, N), func=AF.Sigmoid), [("ps", bg)], [("lnt", oc % 2)])
            self.dve(lambda e, t=t, bp=bp, N=N: e.tensor_tensor(out=t, in0=t, in1=self.psf(bp, N), op=ALU.mult), [("lnt", oc % 2), ("ps", bp)], [("lnt", oc % 2)])
            self.dve(lambda e, t=t, oc=oc, N=N: e.tensor_tensor(out=outf[:, oc, 0:N], in0=t, in1=x2f[:, oc, 0:N], op=ALU.add), [("lnt", oc % 2), tagr + "of"], [tagw + "f"])
            if outb is not None:
                self.pool(lambda e, oc=oc, N=N: e.tensor_copy(out=outb[:, oc, 0:N], in_=outf[:, oc, 0:N]), [tagw + "f"], [tagw + "b"])

    def phase2b(self, tiles, w_d, w_pg, w_pp, w_kvc, w_kvA, w_kvB, x1f_d, hT_d, pT, cosT, sinT, x3f_d, x3b_d, CT_d, KP_d, Ctok_d, ckvT, kpeT,
                KEEP, vcol, vblk, ALPHA):
        c = self.c
        D, DFF, TT, DPLE = c["D"], c["DFF"], c["TT"], c["DPLE"]
        KD, NFC, NPC = D // 128, DFF // 128, DPLE // 128
        self.sb_reset(KEEP)
        Wd = self.T([NFC, D], BF16)
        Wpg = self.T([KD, D], BF16)
        Wpp = self.T([NPC, D], BF16)
        Wkc = self.T([KD, 256], BF16)
        WkA = self.T([KD, 64], BF16)
        WkB = self.T([KD, 64], BF16)
        hT = [self.T([NFC, TT], BF16) for _ in range(2)]
        x1 = [self.T([KD, TT]) for _ in range(2)]
        pf = [self.T([NPC, TT]) for _ in range(2)]
        cs = [self.T([2, TT]) for _ in range(2)]
        pb = self.T([NPC, TT], BF16)
        zf = self.T([KD, TT])
        x2f = self.T([KD, TT])
        x2b = self.T([KD, TT], BF16)
        x3f = self.T([KD, TT])
        x3b = self.T([KD, TT], BF16)
        kvf = self.T([2, TT])
        ckf = self.T([2, TT])
        ckb = self.T([2, TT], BF16)
        kpf = self.T([2, TT])
        kpb = self.T([1, TT], BF16)[:, 0, :]
        ctok = self.T([TT // 128, 256], BF16)
        self.dma(Wd, w_d[:, :, :], [], ["Wd"], q="pool")
        self.dma(Wpg, w_pg[0], [], ["Wpg"], q="pool")
        self.dma(Wpp, w_pp[0], [], ["Wpp"], q="pool")
        self.dma(Wkc, w_kvc[:, :, :], [], ["Wkc"], q="pool")
        self.dma(WkA, w_kvA[:, :, :], [], ["WkA"], q="pool")
        self.dma(WkB, w_kvB[:, :, :], [], ["WkB"], q="pool")

        def load(i):
            kind, s, t0, N, first, last = tiles[i]
            j = i % 2
            self.dma(hT[j][:, :, 0:N], hT_d[:, :, t0:t0 + N], [], [("hT", j)])
            self.dma(x1[j][:, :, 0:N], x1f_d[:, :, t0:t0 + N], [], [("x1", j)])
            self.dma(pf[j][:, :, 0:N], pT[0, :, :, t0:t0 + N], [], [("pf", j)])
            self.dma(cs[j][0:64, 0, 0:N], cosT[:, t0:t0 + N], [], [("cs", j)])
            self.dma(cs[j][0:64, 1, 0:N], sinT[:, t0:t0 + N], [], [("cs", j)])

        load(0)
        for i, (kind, s, t0, N, first, last) in enumerate(tiles):
            if i + 1 < len(tiles):
                load(i + 1)
            j = i % 2
            for k in range(NPC):
                self.pool(lambda e, k=k, j=j, N=N: e.tensor_copy(out=pb[:, k, 0:N], in_=pf[j][:, k, 0:N]), [("pf", j)], ["pb"])
            for oc in range(KD):
                b = self.bank()
                self.mm(self.psf(b, N), [(Wd[:, fc, oc * 128:(oc + 1) * 128], hT[j][:, fc, 0:N]) for fc in range(NFC)], ["Wd", ("hT", j)], [("ps", b)])
                self.dve(lambda e, oc=oc, b=b, j=j, N=N: e.scalar_tensor_tensor(out=zf[:, oc, 0:N], in0=x1[j][:, oc, 0:N], scalar=ALPHA, in1=self.psf(b, N),
                                                                             op0=ALU.mult, op1=ALU.add), [("ps", b), ("x1", j)], ["l0fz"])
            self.layer_norm(zf, KD, N, vblk("ln_ffn_g0", KD), vblk("ln_ffn_b0", KD), x2f, x2b, "l0f")
            self.ple(0, x2f, x2b, pb, Wpg, Wpp, KD, NPC, N, x3f, x3b, "l0f", "x3")
            self.dma(x3f_d[:, :, t0:t0 + N], x3f[:, :, 0:N], ["x3f"], [])
            self.dma(x3b_d[:, :, t0:t0 + N], x3b[:, :, 0:N], ["x3b"], [])
            for cc_ in range(2):
                b = self.bank()
                self.mm(self.psf(b, N), [(Wkc[:, k, cc_ * 128:(cc_ + 1) * 128], x3b[:, k, 0:N]) for k in range(KD)], ["Wkc", "x3b"], [("ps", b)])
                self.act(lambda e, cc_=cc_, b=b, N=N: e.activation(out=kvf[:, cc_, 0:N], in_=self.psf(b, N), func=AF.Identity), [("ps", b)], ["kvz"])
            _, rstd = self.ln_stats([kvf[:, 0, 0:N], kvf[:, 1, 0:N]], N, 1e-6, 256, "kv", rms=True)
            for cc_ in range(2):
                self.dve(lambda e, cc_=cc_, N=N, rstd=rstd: e.tensor_tensor(out=kvf[:, cc_, 0:N], in0=kvf[:, cc_, 0:N], in1=rstd, op=ALU.mult), ["kvz", "st_rstd"], ["kvz"])
                self.act(lambda e, cc_=cc_, N=N: e.activation(out=ckf[:, cc_, 0:N], in_=kvf[:, cc_, 0:N], func=AF.Identity, scale=vcol("kvg", cc_)), ["kvz", "vecs"], ["ckf"])
                self.pool(lambda e, cc_=cc_, N=N: e.tensor_copy(out=ckb[:, cc_, 0:N], in_=ckf[:, cc_, 0:N]), ["ckf"], ["ckb"])
            self.dma(ckvT[:, :, t0:t0 + N], ckf[:, :, 0:N], ["ckf"], [])
            self.dma(CT_d[:, :, t0:t0 + N], ckb[:, :, 0:N], ["ckb"], [])
            bA, bB = self.bank(), self.bank()
            self.mm(self.ps[0:64, bA * 512:bA * 512 + N], [(WkA[:, k, :], x3b[:, k, 0:N]) for k in range(KD)], ["WkA", "x3b"], [("ps", bA)])
            self.mm(self.ps[0:64, bB * 512:bB * 512 + N], [(WkB[:, k, :], x3b[:, k, 0:N]) for k in range(KD)], ["WkB", "x3b"], [("ps", bB)])
            self.dve(lambda e, bA=bA, j=j, N=N: e.tensor_tensor(out=kpf[0:64, 0, 0:N], in0=self.ps[0:64, bA * 512:bA * 512 + N], in1=cs[j][0:64, 0, 0:N], op=ALU.mult),
                     [("ps", bA), ("cs", j)], ["kpf0"])
            self.dve(lambda e, bB=bB, j=j, N=N: e.tensor_tensor(out=kpf[0:64, 1, 0:N], in0=self.ps[0:64, bB * 512:bB * 512 + N], in1=cs[j][0:64, 1, 0:N], op=ALU.mult),
                     [("ps", bB), ("cs", j)], ["kpf1"])
            self.dve(lambda e, N=N: e.tensor_tensor(out=kpf[0:64, 0, 0:N], in0=kpf[0:64, 0, 0:N], in1=kpf[0:64, 1, 0:N], op=ALU.add), ["kpf0", "kpf1"], ["kpf0"])
            self.pool(lambda e, N=N: e.tensor_copy(out=kpb[0:64, 0:N], in_=kpf[0:64, 0, 0:N]), ["kpf0"], ["kpb"])
            self.dma(kpeT[:, t0:t0 + N], kpf[0:64, 0, 0:N], ["kpf0"], [])
            self.dma(KP_d[:, t0:t0 + N], kpb[0:64, 0:N], ["kpb"], [])
            for tk in range(N // 128):
                b = self.bank()
                for cc_ in range(2):
                    self.pe(lambda e, b=b, cc_=cc_, tk=tk: e.transpose(out=self.psb(b)[:, cc_ * 128:(cc_ + 1) * 128], in_=ckb[:, cc_, tk * 128:(tk + 1) * 128],
                                                                     identity=self.identb), ["ckb", "identb"], [("ps", b)])
                self.act(lambda e, b=b, tk=tk: e.activation(out=ctok[:, tk, :], in_=self.psb(b)[:, 0:256], func=AF.Identity), [("ps", b)], ["ctok"])
            self.dma(Ctok_d[:, t0 // 128:t0 // 128 + N // 128, :], ctok[:, 0:N // 128, :], ["ctok"], [])
        self.S.barrier()


    def attn_rowtile(self, Ql, Qpe, groups, OTdst, par):
        st = self.att_st[par]
        m, l, gm, corr, negm, ls, rl = (st[:, i:i + 1] for i in range(7))
        acc = self.att_acc[par]
        sr = ("ast", par)
        for gi, g in enumerate(groups):
            n, kt, nt = g["n"], g["kt"], g["nt"]
            if g.get("prep") is not None:
                g["prep"]()
            bS = self.bank()
            S_ = self.psf(bS, n)
            rr = ["QT"] + g["r"]
            if g["mask"] is None:
                self.mm(S_, [(Ql[0], g["ct"][0]), (Ql[1], g["ct"][1]), (Qpe, g["kp"])], rr, [("ps", bS)])
            else:
                self.mm(S_, [(self.identb, g["mask"]), (Ql[0], g["ct"][0]), (Ql[1], g["ct"][1]), (Qpe, g["kp"])], rr + ["identb", "masks"], [("ps", bS)])
            tgt = m if gi == 0 else gm
            self.dve(lambda e, tgt=tgt, S_=S_: e.reduce_max(out=tgt, in_=S_, axis=AX.X), [("ps", bS)], [sr])
            if gi > 0:
                self.dve(lambda e: e.tensor_tensor(out=gm, in0=gm, in1=m, op=ALU.max), [sr], [sr])
                self.dve(lambda e: e.tensor_tensor(out=corr, in0=m, in1=gm, op=ALU.subtract), [sr], [sr])
                self.dve(lambda e: e.tensor_copy(out=m, in_=gm), [sr], [sr])
                self.act(lambda e: e.activation(out=corr, in_=corr, func=AF.Exp), [sr], [sr])
            self.dve(lambda e: e.tensor_scalar(out=negm, in0=m, scalar1=-1.0, scalar2=None, op0=ALU.mult), [sr], [sr])
            pp = self.bank_i % 2
            P = self.att_P[pp][:, 0:n]
            lt = l if gi == 0 else ls
            self.act(lambda e, P=P, S_=S_, lt=lt: e.activation(out=P, in_=S_, func=AF.Exp, bias=negm, scale=1.0, accum_out=lt), [("ps", bS), sr], [("attP", pp), sr])
            if gi > 0:
                self.dve(lambda e: e.scalar_tensor_tensor(out=l, in0=l, scalar=corr, in1=ls, op0=ALU.mult, op1=ALU.add), [sr], [sr])
            bT = self.bank()
            for i in range(nt):
                self.pe(lambda e, bT=bT, i=i, P=P, kt=kt: e.transpose(out=self.psb(bT)[0:kt, i * 128:(i + 1) * 128], in_=P[:, i * kt:(i + 1) * kt], identity=self.identb),
                        [("attP", pp), "identb"], [("ps", bT)])
            PT = self.att_PT[pp]
            if gi % 2 == 0:
                self.act(lambda e, bT=bT, PT=PT, kt=kt, nt=nt: e.activation(out=PT[0:kt, 0:nt * 128], in_=self.psb(bT)[0:kt, 0:nt * 128], func=AF.Identity), [("ps", bT)], [("attPT", pp)])
            else:
                self.dve(lambda e, bT=bT, PT=PT, kt=kt, nt=nt: e.tensor_copy(out=PT[0:kt, 0:nt * 128], in_=self.psb(bT)[0:kt, 0:nt * 128]), [("ps", bT)], [("attPT", pp)])
            bV = self.bank()
            self.mm(self.psf(bV, 256), [(PT[0:kt, i * 128:(i + 1) * 128], g["ctok"][i]) for i in range(nt)], [("attPT", pp)] + g["r"], [("ps", bV)])
            if gi == 0:
                self.act(lambda e, bV=bV: e.activation(out=acc, in_=self.psf(bV, 256), func=AF.Identity), [("ps", bV)], [sr])
            else:
                self.dve(lambda e, bV=bV: e.scalar_tensor_tensor(out=acc, in0=acc, scalar=corr, in1=self.psf(bV, 256), op0=ALU.mult, op1=ALU.add), [("ps", bV), sr], [sr])
        self.dve(lambda e: e.reciprocal(out=rl, in_=l), [sr], [sr])
        ob_ = self.att_ob[par]
        self.dve(lambda e: e.tensor_scalar(out=ob_, in0=acc, scalar1=rl, scalar2=None, op0=ALU.mult), [sr], [("aob", par)])
        bO = self.bank()
        for cc_ in range(2):
            self.pe(lambda e, bO=bO, cc_=cc_: e.transpose(out=self.psb(bO)[:, cc_ * 128:(cc_ + 1) * 128], in_=ob_[:, cc_ * 128:(cc_ + 1) * 128], identity=self.identb),
                    [("aob", par), "identb"], [("ps", bO)])
        self.act(lambda e, bO=bO: e.activation(out=OTdst, in_=self.psb(bO)[:, 0:256].rearrange("p (c r) -> p c r", c=2), func=AF.Identity), [("ps", bO)], ["OT"])

    def phase4(self, kind, D_, KEEP, vcol, vblk, ALPHA, SCALE):
        c = self.c
        D, SEQ, NSEQ, DB, DS, H, QL, NE = c["D"], c["SEQ"], c["NSEQ"], c["DB"], c["DS"], c["H"], c["QL"], c["NE"]
        KD, QLC = D // 128, QL // 128
        TP = NSEQ * SEQ
        NPG, CHB = c["NPG"], c["CHB"]
        PR = c["NPHYS"] // NCORES
        CP = 1 << CHB
        NCH = PR // CP
        NJ = H * 64 // 128
        self.sb_reset(KEEP)
        Wqa = self.T([KD, QL], BF16)
        WqbN = self.T([QLC, H * 64], BF16)
        WqbA = self.T([QLC, H * 64], BF16)
        WqbB = self.T([QLC, H * 64], BF16)
        WukT = self.T([H // 2, 256], BF16)
        WuvP = self.T([2, H, 128], BF16)
        Wo = self.T([NJ, D], BF16)
        Wr = self.T([KD, NE])
        maskp = self.T([1, 248], BF16)[:, 0, :]
        masks = self.T([1, 256], BF16)[:, 0, :]
        for nm, t_, src in (("Wqa", Wqa, D_["w_qa"]), ("WqbN", WqbN, D_["w_qbN"]), ("WqbA", WqbA, D_["w_qbA"]), ("WqbB", WqbB, D_["w_qbB"]),
                            ("WukT", WukT, D_["w_ukT"]), ("WuvP", WuvP, D_["w_uvP"]), ("Wo", Wo, D_["w_o"])):
            self.dma(t_, src, [], [nm], q="pool")
        self.dma(Wr, D_["w_r"][:, :, :], [], ["Wr"])
        self.dma(maskp, D_["maskp"][:, :], [], ["masks"])
        self.dma(masks, D_["masks"][:, :], [], ["masks"])
        nb_ = 2 if kind == "p" else 1
        x3f = [self.T([KD, 128]) for _ in range(nb_)]
        x3b = [self.T([KD, 128], BF16) for _ in range(nb_)]
        cs = [self.T([2, 128]) for _ in range(nb_)]
        cqf = self.T([QLC, 128])
        cqn = self.T([QLC, 128], BF16)
        qn = self.T([NJ, 128], BF16)
        QTl = self.T([2, 128, H], BF16)
        QTp = self.T([128, H], BF16)
        rt1 = self.T([2, 128])
        OT = self.T([2, 128, H], BF16)
        VT = self.T([NJ, 128], BF16)
        zf = self.T([KD, 128])
        of = self.T([KD, 128])
        ob = self.T([KD, 128], BF16)
        lg = self.T([4, NE])
        self.att_st = [self.T([1, 8])[:, 0, :] for _ in range(2)]
        self.att_acc = [self.T([1, 256])[:, 0, :] for _ in range(2)]
        self.att_ob = [self.T([1, 256], BF16)[:, 0, :] for _ in range(2)]
        self.att_P = [self.T([1, 512], BF16)[:, 0, :] for _ in range(2)]
        self.att_PT = [self.T([1, 512], BF16)[:, 0, :] for _ in range(2)]
        self.pool(lambda e: e.memset(QTp, 0.0), [], ["QT"])
        x3f_d, x3b_d, cosT, sinT = D_["x3f_d"], D_["x3b_d"], D_["cosT"], D_["sinT"]
        CT_d, KP_d, Ctok_d = D_["CT_d"], D_["KP_d"], D_["Ctok_d"]
        if kind == "p":
            CT = self.T([2, SEQ], BF16)
            KP = self.T([1, SEQ], BF16)[:, 0, :]
            CK = self.T([SEQ // 128, 256], BF16)
            self.pool(lambda e: e.memset(KP, 0.0), [], ["KP"])
            qtiles = [(s, qt, s * SEQ + qt * 128) for s in range(NSEQ) for qt in range(SEQ // 128)]
        else:
            CT = self.T([2, 128], BF16)
            KP = self.T([1, 128], BF16)[:, 0, :]
            CK = self.T([1, 256], BF16)
            self.pool(lambda e: e.memset(KP, 0.0), [], ["KP"])
            XcQ = [self.T([max(self.TSLS), 256], BF16) for _ in range(3)]
            XpB = [self.T([128, 32], BF16) for _ in range(2)]
            CTg = [self.T([2, 512], BF16) for _ in range(2)]
            KPg = [self.T([1, 512], BF16)[:, 0, :] for _ in range(2)]
            for i in range(2):
                self.pool(lambda e, i=i: e.memset(KPg[i], 0.0), [], [("KPg", i)])
            ptf = self.T([16, DB])
            pti = self.T([3, DB], I32)
            qtiles = [(0, 0, TP)]
            self.dma(pti[:, 0, :], D_["ptT"][:, :], [], ["pti"])
            self.dma(CT, CT_d[:, :, TP:TP + 128], [], ["CT"])
            self.dma(KP[0:64, :], KP_d[:, TP:TP + 128], ["KP"], ["KP"])
            self.dma(CK, Ctok_d[:, TP // 128:TP // 128 + 1, :], [], ["CK"])
            pf_, q_, r_, t1, t2, t3 = (ptf[:, j, :] for j in range(6))
            self.dve(lambda e: e.tensor_copy(out=pf_, in_=pti[:, 0, :]), ["pti"], ["ix"])
            d = NCORES
            self.dve(lambda e: e.tensor_scalar(out=pti[:, 1, :], in0=pf_, scalar1=1.0 / d, scalar2=None, op0=ALU.mult), ["ix"], ["ix"])
            self.dve(lambda e: e.tensor_copy(out=q_, in_=pti[:, 1, :]), ["ix"], ["ix"])
            self.dve(lambda e: e.scalar_tensor_tensor(out=r_, in0=q_, scalar=-float(d), in1=pf_, op0=ALU.mult, op1=ALU.add), ["ix"], ["ix"])
            self.dve(lambda e: e.tensor_scalar(out=t1, in0=r_, scalar1=0.0, scalar2=None, op0=ALU.is_lt), ["ix"], ["ix"])
            self.dve(lambda e: e.tensor_scalar(out=t2, in0=r_, scalar1=float(d), scalar2=None, op0=ALU.is_ge), ["ix"], ["ix"])
            self.dve(lambda e: e.tensor_tensor(out=t2, in0=t2, in1=t1, op=ALU.subtract), ["ix"], ["ix"])
            self.dve(lambda e: e.tensor_tensor(out=q_, in0=q_, in1=t2, op=ALU.add), ["ix"], ["ix"])
            self.dve(lambda e: e.scalar_tensor_tensor(out=r_, in0=q_, scalar=-float(d), in1=pf_, op0=ALU.mult, op1=ALU.add), ["ix"], ["ix"])
            self.dve(lambda e: e.scalar_tensor_tensor(out=t3, in0=r_, scalar=float(PR), in1=q_, op0=ALU.mult, op1=ALU.add), ["ix"], ["ix"])
            self.dve(lambda e: e.tensor_copy(out=pti[:, 2, :], in_=t3), ["ix"], ["ixk"])
            NS, TSLS, TOFF = self.NS, self.TSLS, self.TOFF
            qcount = [0]

            def issue_upto(n):
                while qcount[0] < min(n, DB * NS):
                    qi = qcount[0]
                    b, k = divmod(qi, NS)
                    buf = qi % 3
                    self.S.add("pool", lambda e, k=k, b=b, buf=buf: e.indirect_dma_start(
                        out=XcQ[buf][0:NPG, 0:TSLS[k], :].rearrange("p t c -> p (t c)"), out_offset=None, in_=D_["cfull"][k][:, :],
                        in_offset=bass.IndirectOffsetOnAxis(ap=pti[0:NPG, 2, b:b + 1], axis=0)),
                        ["ixk", ("cfull", k)], [("Xc", buf)], dma=True)
                    if k == 0:
                        self.S.add("pool", lambda e, b=b: e.indirect_dma_start(
                            out=XpB[b % 2][0:NPG].rearrange("p t c -> p (t c)"), out_offset=None, in_=D_["kfull"][:, :],
                            in_offset=bass.IndirectOffsetOnAxis(ap=pti[0:NPG, 2, b:b + 1], axis=0)), ["ixk", "kfull"], [("Xp", b % 2)], dma=True)
                    qcount[0] += 1

        def load(i):
            s, qt, t0 = qtiles[i]
            j = i % nb_
            self.dma(x3f[j], x3f_d[:, :, t0:t0 + 128], [], [("x3f", j)])
            self.dma(x3b[j], x3b_d[:, :, t0:t0 + 128], [], [("x3b", j)])
            self.dma(cs[j][0:64, 0, :], cosT[:, t0:t0 + 128], [], [("cs", j)])
            self.dma(cs[j][0:64, 1, :], sinT[:, t0:t0 + 128], [], [("cs", j)])

        load(0)
        for i, (s, qt, t0) in enumerate(qtiles):
            if kind == "p" and qt == 0:
                self.dma(CT, CT_d[:, :, s * SEQ:(s + 1) * SEQ], [], ["CT"])
                self.dma(KP[0:64, :], KP_d[:, s * SEQ:(s + 1) * SEQ], ["KP"], ["KP"])
                self.dma(CK, Ctok_d[:, s * SEQ // 128:(s + 1) * SEQ // 128, :], [], ["CK"])
            if i + 1 < len(qtiles):
                load(i + 1)
            j = i % nb_
            Xb, Xf, CS = x3b[j], x3f[j], cs[j]
            for qc in range(QLC):
                b = self.bank()
                self.mm(self.psf(b, 128), [(Wqa[:, k, qc * 128:(qc + 1) * 128], Xb[:, k, :]) for k in range(KD)], ["Wqa", ("x3b", j)], [("ps", b)])
                self.act(lambda e, qc=qc, b=b: e.activation(out=cqf[:, qc, :], in_=self.psf(b, 128), func=AF.Identity), [("ps", b)], ["cqz"])
            _, rstd = self.ln_stats([cqf[:, qc, :] for qc in range(QLC)], 128, 1e-6, QL, "cq", rms=True)
            for qc in range(QLC):
                self.dve(lambda e, qc=qc, rstd=rstd: e.tensor_tensor(out=cqf[:, qc, :], in0=cqf[:, qc, :], in1=rstd, op=ALU.mult), ["cqz", "st_rstd"], ["cqz"])
                self.act(lambda e, qc=qc: e.activation(out=cqn[:, qc, :], in_=cqf[:, qc, :], func=AF.Identity, scale=vcol("qg", qc)), ["cqz", "vecs"], ["cqn"])
            for jj in range(NJ):
                b = self.bank()
                self.mm(self.psf(b, 128), [(WqbN[:, qc, jj * 128:(jj + 1) * 128], cqn[:, qc, :]) for qc in range(QLC)], ["WqbN", "cqn"], [("ps", b)])
                self.act(lambda e, jj=jj, b=b: e.activation(out=qn[:, jj, :], in_=self.psf(b, 128), func=AF.Identity), [("ps", b)], ["qn"])
            for h in range(H):
                bA, bB = self.bank(), self.bank()
                self.mm(self.ps[0:64, bA * 512:bA * 512 + 128], [(WqbA[:, qc, h * 64:(h + 1) * 64], cqn[:, qc, :]) for qc in range(QLC)], ["WqbA", "cqn"], [("ps", bA)])
                self.mm(self.ps[0:64, bB * 512:bB * 512 + 128], [(WqbB[:, qc, h * 64:(h + 1) * 64], cqn[:, qc, :]) for qc in range(QLC)], ["WqbB", "cqn"], [("ps", bB)])
                hp = h % 2
                self.dve(lambda e, bA=bA, CS=CS, hp=hp: e.tensor_tensor(out=rt1[0:64, hp, :], in0=self.ps[0:64, bA * 512:bA * 512 + 128], in1=CS[0:64, 0, :], op=ALU.mult),
                         [("ps", bA), ("cs", j)], [("rt1", hp)])
                self.dve(lambda e, bB=bB, CS=CS, h=h: e.tensor_tensor(out=self.tmp_ln[0:64, 0, 0:128], in0=self.ps[0:64, bB * 512:bB * 512 + 128], in1=CS[0:64, 1, :], op=ALU.mult),
                         [("ps", bB), ("cs", j)], [("lnt", 0)])
                self.dve(lambda e, hp=hp: e.tensor_tensor(out=rt1[0:64, hp, :], in0=rt1[0:64, hp, :], in1=self.tmp_ln[0:64, 0, 0:128], op=ALU.add), [("rt1", hp), ("lnt", 0)], [("rt1", hp)])
                self.act(lambda e, hp=hp, h=h: e.activation(out=QTp[0:64, :, h], in_=rt1[0:64, hp, :], func=AF.Identity, scale=SCALE), [("rt1", hp)], ["QT"])
                bL = self.bank()
                pb_ = (h % 2) * 64
                for cc_ in range(2):
                    self.pe(lambda e, bL=bL, cc_=cc_, h=h, pb_=pb_: e.matmul(self.psf(bL, 256)[:, cc_ * 128:(cc_ + 1) * 128], lhsT=WukT[pb_:pb_ + 64, h // 2, cc_ * 128:(cc_ + 1) * 128],
                                                                       rhs=qn[pb_:pb_ + 64, h // 2, :], start=True, stop=True), ["WukT", "qn"], [("ps", bL)])
                self.act(lambda e, bL=bL, h=h: e.activation(out=QTl[:, :, :, h], in_=self.psf(bL, 256).rearrange("p (c t) -> p c t", c=2), func=AF.Identity, scale=SCALE),
                         [("ps", bL)], ["QT"])
            for rt in range(16):
                Ql = [QTl[:, cc_, rt * 8:(rt + 1) * 8, :].rearrange("p a b -> p (a b)") for cc_ in range(2)]
                Qpe = QTp[0:64, rt * 8:(rt + 1) * 8, :].rearrange("p a b -> p (a b)")
                groups = []
                if kind == "p":
                    full = list(range(qt))
                    for g0 in range(0, len(full), 4):
                        tl = full[g0:g0 + 4]
                        a, bnd = tl[0] * 128, (tl[-1] + 1) * 128
                        groups.append(dict(n=bnd - a, kt=128, nt=len(tl), ct=[CT[:, 0, a:bnd], CT[:, 1, a:bnd]], kp=KP[0:64, a:bnd], ctok=[CK[:, t_, :] for t_ in tl],
                                           mask=None, r=["CT", "KP", "CK"]))
                    a = qt * 128
                    groups.append(dict(n=128, kt=128, nt=1, ct=[CT[:, 0, a:a + 128], CT[:, 1, a:a + 128]], kp=KP[0:64, a:a + 128], ctok=[CK[:, qt, :]],
                                       mask=maskp[:, 120 - 8 * rt:248 - 8 * rt], r=["CT", "KP", "CK"]))
                else:
                    b_ = rt
                    gcnt = 0
                    for k in range(NS):
                      for g0 in range(0, TSLS[k], 4):
                        nt_ = min(4, TSLS[k] - g0)
                        gp = gcnt % 2
                        gcnt += 1
                        qi = b_ * NS + k
                        buf = qi % 3
                        Xc = XcQ[buf]
                        Xp = XpB[b_ % 2]

                        def prep(g0=g0, gp=gp, qi=qi, buf=buf, Xc=Xc, Xp=Xp, b_=b_, nt_=nt_, k=k):
                            if g0 == 0:
                                issue_upto(qi + 3)
                            bC = self.bank()
                            for ti in range(nt_):
                                for cc_ in range(2):
                                    self.pe(lambda e, bC=bC, ti=ti, cc_=cc_: e.transpose(out=self.psb(bC)[:, (cc_ * 4 + ti) * NPG:(cc_ * 4 + ti + 1) * NPG],
                                                                                   in_=Xc[0:NPG, g0 + ti, cc_ * 128:(cc_ + 1) * 128], identity=self.identb[0:NPG, 0:NPG]),
                                            [("Xc", buf), "identb"], [("ps", bC)])
                            self.act(lambda e, bC=bC, gp=gp: e.activation(out=CTg[gp][:, :, 0:nt_ * NPG], in_=self.psb(bC)[:, 0:8 * NPG].rearrange("p (c n) -> p c n", c=2)[:, :, 0:nt_ * NPG],
                                                                          func=AF.Identity), [("ps", bC)], [("CTg", gp)])
                            bK = self.bank()
                            for ti in range(nt_):
                                t_ = TOFF[k] + g0 + ti
                                for hf in range(2):
                                    self.pe(lambda e, bK=bK, ti=ti, t_=t_, hf=hf: e.transpose(out=self.psb(bK)[hf * 32:hf * 32 + 16, ti * NPG:(ti + 1) * NPG],
                                                                                        in_=Xp[0:NPG, t_, hf * 16:(hf + 1) * 16], identity=self.identb[0:NPG, 0:NPG]),
                                            [("Xp", b_ % 2), "identb"], [("ps", bK)])
                            self.dve(lambda e, bK=bK, gp=gp: e.tensor_copy(out=KPg[gp][0:16, 0:nt_ * NPG], in_=self.psb(bK)[0:16, 0:nt_ * NPG]), [("ps", bK)], [("KPg", gp)])
                            self.dve(lambda e, bK=bK, gp=gp: e.tensor_copy(out=KPg[gp][32:48, 0:nt_ * NPG], in_=self.psb(bK)[32:48, 0:nt_ * NPG]), [("ps", bK)], [("KPg", gp)])

                        groups.append(dict(n=nt_ * NPG, kt=NPG, nt=nt_, ct=[CTg[gp][:, 0, 0:nt_ * NPG], CTg[gp][:, 1, 0:nt_ * NPG]], kp=KPg[gp][0:64, 0:nt_ * NPG],
                                           ctok=[Xc[0:NPG, g0 + ti, :] for ti in range(nt_)], mask=None, r=[("CTg", gp), ("KPg", gp), ("Xc", buf)], prep=prep))
                    groups.append(dict(n=128, kt=128, nt=1, ct=[CT[:, 0, :], CT[:, 1, :]], kp=KP[0:64, :], ctok=[CK[:, 0, :]],
                                       mask=masks[:, 120 - 8 * b_:248 - 8 * b_], r=["CT", "KP", "CK"]))
                self.attn_rowtile(Ql, Qpe, groups, OT[:, :, rt * 8:(rt + 1) * 8, :].rearrange("p c a b -> p c (a b)"), rt % 2)
            for jj in range(NJ):
                b = self.bank()
                prs = []
                for hh in range(2):
                    h = 2 * jj + hh
                    for cc_ in range(2):
                        prs.append((WuvP[:, cc_, h, :], OT[:, cc_, :, h]))
                self.mm(self.psf(b, 128), prs, ["WuvP", "OT"], [("ps", b)])
                self.act(lambda e, jj=jj, b=b: e.activation(out=VT[:, jj, :], in_=self.psf(b, 128), func=AF.Identity), [("ps", b)], ["VT"])
            for oc in range(KD):
                b = self.bank()
                self.mm(self.psf(b, 128), [(Wo[:, jj, oc * 128:(oc + 1) * 128], VT[:, jj, :]) for jj in range(NJ)], ["Wo", "VT"], [("ps", b)])
                self.dve(lambda e, oc=oc, b=b, Xf=Xf: e.scalar_tensor_tensor(out=zf[:, oc, :], in0=Xf[:, oc, :], scalar=ALPHA, in1=self.psf(b, 128), op0=ALU.mult, op1=ALU.add),
                         [("ps", b), ("x3f", j)], ["l1z"])
            self.layer_norm(zf, KD, 128, vblk("ln_mix_g1", KD), vblk("ln_mix_b1", KD), of, ob, "l1")
            self.dma(D_["x4f_d"][:, :, t0:t0 + 128], of, ["l1of"], [])
            self.dma(D_["x4b_d"][:, :, t0:t0 + 128], ob, ["l1ob"], [])
            b = self.bank()
            self.mm(self.psf(b, NE), [(of[:, k, :], Wr[:, k, :]) for k in range(KD)], ["l1of", "Wr"], [("ps", b)])
            L0, L8, L2, L3 = (lg[:, q, :] for q in range(4))
            self.act(lambda e, b=b: e.activation(out=L0, in_=self.psf(b, NE), func=AF.Identity), [("ps", b)], ["lg"])
            self.dve(lambda e: e.max(out=L8, in_=L0), ["lg"], ["lg8"])
            self.dve(lambda e: e.tensor_tensor(out=L3[:, 0:1], in0=L8[:, 0:1], in1=L8[:, 1:2], op=ALU.add), ["lg8"], ["lg3"])
            self.dve(lambda e: e.tensor_scalar(out=L2, in0=L0, scalar1=2.0, scalar2=L3[:, 0:1], op0=ALU.mult, op1=ALU.subtract), ["lg", "lg3"], ["lg2"])
            self.act(lambda e: e.activation(out=L2, in_=L2, func=AF.Sigmoid), ["lg2"], ["lg2"])
            self.dve(lambda e: e.tensor_scalar(out=L0, in0=L0, scalar1=L8[:, 1:2], scalar2=None, op0=ALU.is_ge), ["lg", "lg8"], ["lg"])
            self.dve(lambda e: e.tensor_tensor(out=L2, in0=L2, in1=L0, op=ALU.mult), ["lg", "lg2"], ["lg2"])
            self.dma(D_["gates_d"][:, t0 // 128, :], L2, ["lg2"], [])
        self.S.barrier()


    def phase5(self, D_, KEEP):
        c = self.c
        D, NE, DFFE = c["D"], c["NE"], c["DFFE"]
        KD = D // 128
        NG = DFFE // 512
        T = self.Ttok
        NT128 = T // 128
        GW, DWG = KD * 512, 4 * D
        GRP = 2 * GW + DWG
        DH = min(512, D)
        ND = D // DH
        moe_full, x4b_d, gates_d, moeT_d = D_["moe_full"], D_["x4b_d"], D_["gates_d"], D_["moeT_d"]
        halves = [list(range(0, NT128 // 2)), list(range(NT128 // 2, NT128))]
        NTH = max(len(h) for h in halves)
        for hi, tl in enumerate(halves):
            self.sb_reset(KEEP)
            nt = len(tl)
            t0, Th = tl[0] * 128, nt * 128
            xb = self.T([KD, NTH * 128], BF16)
            gt = self.T([NTH, NE])
            acc = self.T([NTH, D])
            Wc = [self.T([1, GRP], BF16)[:, 0, :] for _ in range(2)]
            hT = [self.T([4, 512], BF16) for _ in range(2)]
            sg = [self.T([1, 512])[:, 0, :] for _ in range(2)]
            moT = [self.T([KD, 128]) for _ in range(2)]
            self.dma(xb[:, :, 0:Th], x4b_d[:, :, t0:t0 + Th], [], ["xb"])
            self.dma(gt[:, 0:nt, :], gates_d[:, tl[0]:tl[0] + nt, :], [], ["gt"])
            subt = [(a, min(512, Th - a)) for a in range(0, Th, 512)]
            gi = 0
            for e_ in range(NE):
                for g in range(NG):
                    wi = gi % 2
                    gi += 1
                    W = Wc[wi]
                    self.dma(W, moe_full[e_ * 128:(e_ + 1) * 128, g * GRP:(g + 1) * GRP], ["moe_full"], [("Wc", wi)])
                    Wg = W[:, 0:GW].rearrange("p (k n) -> p k n", k=KD)
                    Wu = W[:, GW:2 * GW].rearrange("p (k n) -> p k n", k=KD)
                    Wd = W[:, 2 * GW:GRP].rearrange("p (f n) -> p f n", f=4)
                    for si, (a, n) in enumerate(subt):
                        hp = si % 2
                        Hh = hT[hp]
                        for f in range(4):
                            bg, bu = self.bank(), self.bank()
                            self.mm(self.psf(bg, n), [(Wg[:, k, f * 128:(f + 1) * 128], xb[:, k, a:a + n]) for k in range(KD)], [("Wc", wi), "xb"], [("ps", bg)])
                            self.mm(self.psf(bu, n), [(Wu[:, k, f * 128:(f + 1) * 128], xb[:, k, a:a + n]) for k in range(KD)], [("Wc", wi), "xb"], [("ps", bu)])
                            sgt = sg[f % 2][:, 0:n]
                            self.act(lambda e, sgt=sgt, bg=bg, n=n: e.activation(out=sgt, in_=self.psf(bg, n), func=AF.Silu), [("ps", bg)], [("sg", f % 2)])
                            self.dve(lambda e, sgt=sgt, bu=bu, n=n, f=f, Hh=Hh: e.tensor_tensor(out=Hh[:, f, 0:n], in0=sgt, in1=self.psf(bu, n), op=ALU.mult),
                                     [("sg", f % 2), ("ps", bu)], [("hT", hp)])
                        for tk in range(n // 128):
                            ti = a // 128 + tk
                            for dh in range(ND):
                                bo = self.bank()
                                self.mm(self.psf(bo, DH), [(Hh[:, f, tk * 128:(tk + 1) * 128], Wd[:, f, dh * DH:(dh + 1) * DH]) for f in range(4)],
                                        [("hT", hp), ("Wc", wi)], [("ps", bo)])
                                dst = acc[:, ti, dh * DH:(dh + 1) * DH]
                                gcol = gt[:, ti, e_:e_ + 1]
                                if e_ == 0 and g == 0:
                                    self.dve(lambda e, dst=dst, bo=bo, gcol=gcol: e.tensor_scalar(out=dst, in0=self.psf(bo, DH), scalar1=gcol, scalar2=None, op0=ALU.mult),
                                             [("ps", bo), "gt"], [("acc", ti)])
                                else:
                                    self.dve(lambda e, dst=dst, bo=bo, gcol=gcol: e.scalar_tensor_tensor(out=dst, in0=self.psf(bo, DH), scalar=gcol, in1=dst, op0=ALU.mult, op1=ALU.add),
                                             [("ps", bo), "gt", ("acc", ti)], [("acc", ti)])
            for ti in range(nt):
                mp = ti % 2
                for k0 in range(0, KD, 4):
                    kn = min(4, KD - k0)
                    b = self.bank()
                    for k in range(kn):
                        self.pe(lambda e, b=b, k=k, k0=k0, ti=ti: e.transpose(out=self.psf(b)[:, k * 128:(k + 1) * 128], in_=acc[:, ti, (k0 + k) * 128:(k0 + k + 1) * 128], identity=self.identf),
                                [("acc", ti), "identf"], [("ps", b)])
                    self.act(lambda e, b=b, k0=k0, kn=kn, mp=mp: e.activation(out=moT[mp][:, k0:k0 + kn, :], in_=self.psf(b, kn * 128).rearrange("p (k t) -> p k t", k=kn), func=AF.Identity),
                             [("ps", b)], [("moT", mp)])
                self.dma(moeT_d[:, :, t0 + ti * 128:t0 + (ti + 1) * 128], moT[mp], [("moT", mp)], [])
            self.S.barrier()

    def phase6(self, tiles, D_, KEEP, vcol, vblk, ALPHA):
        c = self.c
        D, TT, DPLE = c["D"], c["TT"], c["DPLE"]
        KD, NPC = D // 128, DPLE // 128
        self.sb_reset(KEEP)
        Wpg = self.T([KD, D], BF16)
        Wpp = self.T([NPC, D], BF16)
        x4 = [self.T([KD, TT]) for _ in range(2)]
        mo = [self.T([KD, TT]) for _ in range(2)]
        pf = [self.T([NPC, TT]) for _ in range(2)]
        pb = self.T([NPC, TT], BF16)
        zf = self.T([KD, TT])
        x5f = self.T([KD, TT])
        x5b = self.T([KD, TT], BF16)
        yf = self.T([KD, TT])
        self.dma(Wpg, D_["w_pg"][1], [], ["Wpg"], q="pool")
        self.dma(Wpp, D_["w_pp"][1], [], ["Wpp"], q="pool")

        def load(i):
            kind, s, t0, N, first, last = tiles[i]
            j = i % 2
            self.dma(x4[j][:, :, 0:N], D_["x4f_d"][:, :, t0:t0 + N], [], [("x4", j)])
            self.dma(mo[j][:, :, 0:N], D_["moeT_d"][:, :, t0:t0 + N], [], [("mo", j)])
            self.dma(pf[j][:, :, 0:N], D_["pT"][1, :, :, t0:t0 + N], [], [("pf", j)])

        load(0)
        for i, (kind, s, t0, N, first, last) in enumerate(tiles):
            if i + 1 < len(tiles):
                load(i + 1)
            j = i % 2
            for k in range(NPC):
                self.pool(lambda e, k=k, j=j, N=N: e.tensor_copy(out=pb[:, k, 0:N], in_=pf[j][:, k, 0:N]), [("pf", j)], ["pb"])
            for oc in range(KD):
                self.dve(lambda e, oc=oc, j=j, N=N: e.scalar_tensor_tensor(out=zf[:, oc, 0:N], in0=x4[j][:, oc, 0:N], scalar=ALPHA, in1=mo[j][:, oc, 0:N], op0=ALU.mult, op1=ALU.add),
                         [("x4", j), ("mo", j)], ["l1fz"])
            self.layer_norm(zf, KD, N, vblk("ln_ffn_g1", KD), vblk("ln_ffn_b1", KD), x5f, x5b, "l1f")
            self.ple(1, x5f, x5b, pb, Wpg, Wpp, KD, NPC, N, yf, None, "l1f", "y")
            self.dma(D_["yT"][:, :, t0:t0 + N], yf[:, :, 0:N], ["yf"], [])
        self.S.barrier()

def prep_inputs(cfg, I):
    c = cfg
    D, SEQ, NSEQ, DB, DS = c["D"], c["SEQ"], c["NSEQ"], c["DB"], c["DS"]
    KD = D // 128
    H = c["H"]
    TP, TS = NSEQ * SEQ, DB * DS
    T = TP + TS
    NRC = c["DRNN"] // 128
    f32 = np.float32
    bf = ml_dtypes.bfloat16
    inv = (c["THETA"] ** (-np.arange(0, 32, 2, dtype=f32) / f32(32))).astype(f32)
    pos = np.concatenate([np.tile(np.arange(SEQ, dtype=f32), NSEQ), np.tile(c["PAST"] + np.arange(DS, dtype=f32), DB)]).astype(f32)
    ang = (pos[:, None] * inv[None, :]).astype(f32)
    cos, sin = np.cos(ang).astype(f32).T, np.sin(ang).astype(f32).T
    cosT = np.zeros((64, T), f32)
    sinT = np.zeros((64, T), f32)
    cosT[0:16], cosT[32:48] = cos, cos
    sinT[0:16], sinT[32:48] = -sin, sin
    r = np.arange(128)
    ql = r // 16
    j = np.arange(248)
    maskp = np.where(j[None, :] <= ql[:, None] + 120, 0.0, NEG).astype(bf)
    j2 = np.arange(256)
    masks = np.where((j2[None, :] >= 120) & (j2[None, :] < 128) & (j2[None, :] - 120 <= ql[:, None]), 0.0, NEG).astype(bf)
    identb = np.eye(128, dtype=f32).astype(bf)
    identf = np.eye(128, dtype=f32)
    W = {k: np.asarray(v) for k, v in I.items()}
    vec_parts = [colv(W["ln_mix_g"][0]), colv(W["ln_mix_b"][0]), colv(W["ln_ffn_g"][0]), colv(W["ln_ffn_b"][0]),
                 colv(W["ln_mix_g"][1]), colv(W["ln_mix_b"][1]), colv(W["ln_ffn_g"][1]), colv(W["ln_ffn_b"][1]),
                 np.ascontiguousarray(W["rg_conv_w"][0].T.reshape(NRC, 128, 4).transpose(1, 0, 2).reshape(128, NRC * 4)),
                 colv(W["rg_conv_b"][0]), colv(W["rg_b_a"][0]), colv(W["rg_b_i"][0]), colv(W["rg_lambda"][0]),
                 colv(W["kv_norm_g"]), colv(W["mla_q_norm_g"][0])]
    vecs = np.ascontiguousarray(np.concatenate(vec_parts, axis=1).astype(f32))
    kva = W["kv_w_a"]
    qb = W["mla_w_q_b"][0].reshape(c["QL"], H, 96)
    qbN = qb[:, :, 0:64].reshape(c["QL"], H * 64)
    qbA = np.concatenate([rope_pad_cols(qb[:, h, 64:96], False) for h in range(H)], axis=1)
    qbB = np.concatenate([rope_pad_cols(qb[:, h, 64:96], True) for h in range(H)], axis=1)
    uk = W["kv_w_uk"]
    ukT = np.zeros((128, H // 2, 256), f32)
    for h in range(H):
        ukT[(h % 2) * 64:(h % 2) * 64 + 64, h // 2, :] = uk[:, h, :].T
    uv = W["kv_w_uv"]
    uvP = np.zeros((128, 2, H, 128), f32)
    for h in range(H):
        for cc_ in range(2):
            uvP[:, cc_, h, (h % 2) * 64:(h % 2) * 64 + 64] = uv[cc_ * 128:(cc_ + 1) * 128, h, :]
    shared = dict(
        cosT=cosT, sinT=sinT, maskp=maskp, masks=masks, identb=identb, identf=identf, vecs=vecs,
        w_rg_g=kmaj(W["rg_w_gate"][0]), w_rg_x=kmaj(W["rg_w_x"][0]),
        w_rg_a=np.ascontiguousarray(W["rg_w_a"][0].transpose(1, 0, 2)), w_rg_i=np.ascontiguousarray(W["rg_w_i"][0].transpose(1, 0, 2)),
        w_rg_o=kmaj(W["rg_w_out"][0]),
        w_f_g=kmaj(W["ffn_w_gate"][0]), w_f_u=kmaj(W["ffn_w_up"][0]), w_f_d=kmaj(W["ffn_w_down"][0]),
        w_pg=np.stack([kmaj(W["ple_w_gate"][i]) for i in range(2)]), w_pp=np.stack([kmaj(W["ple_w_proj"][i]) for i in range(2)]),
        w_kvc=kmaj(kva[:, 0:256]), w_kvA=kmaj(rope_pad_cols(kva[:, 256:288], False)), w_kvB=kmaj(rope_pad_cols(kva[:, 256:288], True)),
        w_qa=kmaj(W["mla_w_q_a"][0]), w_qbN=kmaj(qbN), w_qbA=kmaj(qbA), w_qbB=kmaj(qbB),
        w_ukT=ukT, w_uvP=uvP, w_o=kmaj(W["mla_w_o"][0]), w_r=kmaj(W["moe_w_router"][0]),
    )
    NG = c["DFFE"] // 512
    maps = []
    for core in range(NCORES):
        m = dict(shared)
        xp = W["x_prompt"][core * NSEQ:(core + 1) * NSEQ].reshape(TP, D)
        xs = W["x_sample"][core * DB:(core + 1) * DB].reshape(TS, D)
        x = np.concatenate([xp, xs], 0)
        m["xT"] = np.ascontiguousarray(x.T.reshape(KD, 128, T).transpose(1, 0, 2))
        pp = W["p_prompt"][:, core * NSEQ:(core + 1) * NSEQ].reshape(2, TP, -1)
        psm = W["p_sample"][:, core * DB:(core + 1) * DB].reshape(2, TS, -1)
        p = np.concatenate([pp, psm], 1)
        m["pT"] = np.ascontiguousarray(p.transpose(0, 2, 1).reshape(2, -1, 128, T).transpose(0, 2, 1, 3))
        sc = W["state_conv"][0, core * DB:(core + 1) * DB]
        m["cst"] = np.ascontiguousarray(sc.transpose(2, 0, 1).reshape(NRC, 128, DB, 3).transpose(1, 0, 2, 3))
        sr = W["state_rnn"][0, core * DB:(core + 1) * DB]
        m["hst"] = np.ascontiguousarray(sr.T.reshape(NRC, 128, DB).transpose(1, 0, 2))
        m["ckv_sh"] = np.ascontiguousarray(W["cache_ckv"][core::NCORES]).reshape(-1, 256)
        m["kpe_sh"] = np.ascontiguousarray(W["cache_kpe"][core::NCORES]).reshape(-1, 32)
        pt = W["page_table"][core * DB:(core + 1) * DB].astype(np.int32)
        ptT = np.zeros((128, DB), np.int32)
        ptT[0:pt.shape[1], :] = pt.T
        m["ptT"] = ptT
        e = core
        parts = []
        wg = W["moe_w_gate"][0][e].reshape(KD, 128, NG, 512).transpose(2, 1, 0, 3).reshape(NG, 128, KD * 512)
        wu = W["moe_w_up"][0][e].reshape(KD, 128, NG, 512).transpose(2, 1, 0, 3).reshape(NG, 128, KD * 512)
        wd = W["moe_w_down"][0][e].reshape(NG, 4, 128, D).transpose(0, 2, 1, 3).reshape(NG, 128, 4 * D)
        m["moe_sh"] = np.ascontiguousarray(np.concatenate([wg, wu, wd], axis=2).transpose(1, 0, 2).reshape(128, -1))
        maps.append(m)
    moe_all = np.concatenate([m["moe_sh"] for m in maps], 0)
    for m in maps:
        m["moe_all"] = moe_all
    if c.get("NOCC"):
        ckv_all = np.concatenate([m["ckv_sh"] for m in maps], 0)
        kpe_all = np.concatenate([m["kpe_sh"] for m in maps], 0)
        for m in maps:
            m["ckv_all"], m["kpe_all"] = ckv_all, kpe_all
    return maps


_CACHE = {}


def get_program(cfg):
    key = tuple(sorted(cfg.items()))
    if key not in _CACHE:
        b = Builder(cfg)
        es = b.build()
        b.S.finalize()
        b.S.emit(b.nc, es)
        es.close()
        _CACHE[key] = b
    return _CACHE[key]


def run_cfg(cfg, I):
    b = get_program(cfg)
    maps = prep_inputs(cfg, I)
    for m in maps:
        for k in list(m.keys()):
            if k not in b.din:
                del m[k]
    res = run_bass_kernel_spmd(b.nc, maps, core_ids=list(range(NCORES)))
    return b, res.results


def kernel(**inputs):
    cfg = full_cfg()
    b, R = run_cfg(cfg, inputs)
    return assemble(cfg, R)


def assemble(cfg, R):
    c = cfg
    D, SEQ, NSEQ, DB, DS = c["D"], c["SEQ"], c["NSEQ"], c["DB"], c["DS"]
    TP = NSEQ * SEQ
    DRNN = c["DRNN"]

    def fm(a):
        return a.transpose(1, 0, *range(2, a.ndim)).reshape(a.shape[0] * a.shape[1], *a.shape[2:])

    yp, ys, cvp, rnp, ckp, kpp, cvs, rns, cks, kps = [], [], [], [], [], [], [], [], [], []
    for r in R:
        y = fm(r["yT"]).T
        yp.append(y[:TP].reshape(NSEQ, SEQ, D))
        ys.append(y[TP:].reshape(DB, DS, D))
        cvp.append(fm(r["conv_p"]).transpose(1, 2, 0))
        rnp.append(r["rnn_p"].transpose(1, 2, 0).reshape(NSEQ, DRNN))
        cvs.append(fm(r["conv_s"]).transpose(1, 2, 0))
        rns.append(fm(r["rnn_s"]).T)
        ck = fm(r["ckvT"]).T
        ckp.append(ck[:TP].reshape(NSEQ, SEQ, 256))
        cks.append(ck[TP:].reshape(DB, DS, 256))
        kp = np.concatenate([r["kpeT"][0:16], r["kpeT"][32:48]], 0).T
        kpp.append(kp[:TP].reshape(NSEQ, SEQ, 32))
        kps.append(kp[TP:].reshape(DB, DS, 32))
    cat = lambda l: np.ascontiguousarray(np.concatenate(l, 0).astype(np.float32))
    return (cat(yp), cat(ys), cat(cvp)[None], cat(rnp)[None], cat(ckp), cat(kpp), cat(cvs)[None], cat(rns)[None], cat(cks), cat(kps))
```
